# Optimizing a Trainium2 kernel written in Bass

```python
import math
import jax
import jax.numpy as jnp
from jax import lax
import numpy as np

D_MODEL = 1024
BATCH = 4
SEQ = 8192
DEPTH = 1

MEM_LEN = 256
DA_HEADS = 4
DA_HEAD_DIM = 64
DA_V_DIM = 2 * DA_HEAD_DIM
DA_QK_WIDTH = DA_HEADS * 2 * DA_HEAD_DIM
DA_WIDTH = DA_HEADS * DA_V_DIM
RW_HEADS = 4
RW_HEAD_DIM = 64
RW_WIDTH = RW_HEADS * RW_HEAD_DIM
RW_DECAY_LORA = 64
RW_AAA_LORA = 64
RW_GATE_LORA = 128
RW_COLS = 3 * RW_WIDTH + RW_DECAY_LORA + RW_AAA_LORA + RW_GATE_LORA
MEM_HEADS = 4
MEM_HEAD_DIM = 64
MEM_WIDTH = MEM_HEADS * MEM_HEAD_DIM
D_MIX = DA_WIDTH + RW_WIDTH + MEM_WIDTH
D_IN = 2 * DA_QK_WIDTH + DA_WIDTH + RW_COLS + MEM_WIDTH
Q_BLOCK = 128
N_GROUPS = 4
EXPERTS_PER_GROUP = 4
N_EXPERTS = N_GROUPS * EXPERTS_PER_GROUP
TOP_K_IN_GROUP = 2
D_EXPERT = 512
RMS_EPS = 1e-6
RW_GN_EPS = 64e-5
MASK_VALUE = -1e30

kernel_name = 'hymba_diffattn_rwkv7_hmoe_layer'


def rms_norm(x, gain, eps=RMS_EPS):
    xf = x.astype(jnp.float32)
    y = xf * lax.rsqrt(jnp.mean(xf * xf, axis=-1, keepdims=True) + eps)
    return (y * gain.astype(jnp.float32)).astype(x.dtype)


def alibi_slopes(n_heads):
    return jnp.asarray([2.0 ** (-8.0 * (i + 1) / n_heads) for i in range(n_heads)], dtype=jnp.float32)


def diff_attention(q, k, v, positions, q_gain, k_gain, lam, lam_init, sub_gain):
    B, S, _ = q.shape
    H, d = DA_HEADS, DA_HEAD_DIM
    nb = S // Q_BLOCK
    q = rms_norm(q.reshape(B, S, H, 2, d), q_gain) * (d ** -0.5)
    k = rms_norm(k.reshape(B, S, H, 2, d), k_gain)
    qh = jnp.transpose(q, (0, 2, 3, 1, 4))
    kh = jnp.transpose(k, (0, 2, 3, 1, 4))
    vh = jnp.transpose(v.reshape(B, S, H, DA_V_DIM), (0, 2, 1, 3))
    q_blocks = jnp.moveaxis(qh.reshape(B, H, 2, nb, Q_BLOCK, d), 3, 0)
    pos_blocks = jnp.transpose(positions.reshape(B, nb, Q_BLOCK), (1, 0, 2))
    idx_blocks = jnp.arange(S, dtype=jnp.int32).reshape(nb, Q_BLOCK)
    k_idx = jnp.arange(S, dtype=jnp.int32)
    slopes = alibi_slopes(H)

    def block(args):
        qb, pb, ib = args
        s = jnp.einsum('bhmqd,bhmkd->bhmqk', qb, kh).astype(jnp.float32)
        dist = (pb[:, :, None] - positions[:, None, :]).astype(jnp.float32)
        bias = -slopes[None, :, None, None, None] * dist[:, None, None, :, :]
        causal = ib[:, None] >= k_idx[None, :]
        p = jax.nn.softmax(jnp.where(causal, s + bias, MASK_VALUE), axis=-1)
        attn = p[:, :, 0] - lam * p[:, :, 1]
        return jnp.einsum('bhqk,bhke->bhqe', attn.astype(vh.dtype), vh)

    out = lax.map(block, (q_blocks, pos_blocks, idx_blocks))
    out = jnp.transpose(out, (1, 0, 3, 2, 4)).reshape(B, S, H, DA_V_DIM)
    out = rms_norm(out, sub_gain) * (1.0 - lam_init)
    return out.reshape(B, S, DA_WIDTH)


def rwkv7_step(state, inp):
    r_t, w_t, k_t, v_t, kk_t, a_t = inp
    sa = jnp.einsum('bhvk,bhk->bhv', state, -kk_t)
    state = (state * w_t[:, :, None, :]
             + sa[..., None] * (kk_t * a_t)[:, :, None, :]
             + v_t[..., None] * k_t[:, :, None, :])
    y = jnp.einsum('bhvk,bhk->bhv', state, r_t)
    return state, y


def rwkv7_time_mix(cols, mu, w0, w2, a0, a2, g2, k_k, k_a, r_k, gn_w, gn_b):
    B, S, _ = cols.shape
    H, N = RW_HEADS, RW_HEAD_DIM
    prev = jnp.pad(cols, ((0, 0), (1, 0), (0, 0)))[:, :S]
    cols = cols + (prev - cols) * mu
    o1 = RW_WIDTH
    o2 = 2 * RW_WIDTH
    o3 = 3 * RW_WIDTH
    o4 = o3 + RW_DECAY_LORA
    o5 = o4 + RW_AAA_LORA
    r, k, v, xw, xa, xg = jnp.split(cols, [o1, o2, o3, o4, o5], axis=-1)
    w = -jax.nn.softplus(-(w0 + jnp.tanh(xw) @ w2)) - 0.5
    a = jax.nn.sigmoid(a0 + xa @ a2)
    g = jax.nn.sigmoid(xg) @ g2

    def heads(t):
        return t.reshape(B, S, H, N).astype(jnp.float32)

    r, k, v, w, a = heads(r), heads(k), heads(v), heads(w), heads(a)
    kk = k * k_k.reshape(H, N).astype(jnp.float32)
    kk = kk / jnp.maximum(jnp.sqrt(jnp.sum(kk * kk, axis=-1, keepdims=True)), 1e-12)
    k = k * (1.0 + (a - 1.0) * k_a.reshape(H, N).astype(jnp.float32))
    decay = jnp.exp(-jnp.exp(w))

    def time_major(t):
        return jnp.transpose(t, (1, 0, 2, 3))

    state0 = jnp.zeros((B, H, N, N), jnp.float32)
    _, y = lax.scan(rwkv7_step, state0,
                    (time_major(r), time_major(decay), time_major(k), time_major(v), time_major(kk), time_major(a)))
    y = jnp.transpose(y, (1, 0, 2, 3))
    mean = jnp.mean(y, axis=-1, keepdims=True)
    var = jnp.mean(jnp.square(y - mean), axis=-1, keepdims=True)
    y = (y - mean) * lax.rsqrt(var + RW_GN_EPS)
    y = y * gn_w.reshape(H, N).astype(jnp.float32) + gn_b.reshape(H, N).astype(jnp.float32)
    y = y + jnp.sum(r * k * r_k.astype(jnp.float32), axis=-1, keepdims=True) * v
    return (y.reshape(B, S, RW_WIDTH) * g.astype(jnp.float32)).astype(cols.dtype)


def memory_attention(q, mem_n, w_kv, q_gain, k_gain):
    B, S, _ = q.shape
    M = mem_n.shape[1]
    mk, mv = jnp.split(mem_n @ w_kv, 2, axis=-1)
    q = rms_norm(q.reshape(B, S, MEM_HEADS, MEM_HEAD_DIM), q_gain)
    mk = rms_norm(mk.reshape(B, M, MEM_HEADS, MEM_HEAD_DIM), k_gain)
    mv = mv.reshape(B, M, MEM_HEADS, MEM_HEAD_DIM)
    s = jnp.einsum('bshd,bmhd->bhsm', q, mk).astype(jnp.float32) * (MEM_HEAD_DIM ** -0.5)
    p = jax.nn.softmax(s, axis=-1)
    o = jnp.einsum('bhsm,bmhd->bshd', p.astype(mv.dtype), mv)
    return o.reshape(B, S, MEM_WIDTH)


def hierarchical_moe(xn, w_gr, b_gr, w_er, b_er, w_gate, w_up, w_down):
    B, S, D = xn.shape
    T = B * S
    xt = xn.reshape(T, D)
    rows = jnp.arange(T)
    g_logits = (xt @ w_gr + b_gr).astype(jnp.float32)
    p_g = jax.nn.softmax(g_logits, axis=-1)
    g_idx = jnp.argmax(g_logits, axis=-1)
    p_sel = p_g[rows, g_idx]
    e_logits = (xt @ w_er + b_er).astype(jnp.float32).reshape(T, N_GROUPS, EXPERTS_PER_GROUP)
    e_logits = e_logits[rows, g_idx]
    top_v, top_i = lax.top_k(e_logits, TOP_K_IN_GROUP)
    wts = jax.nn.softmax(top_v, axis=-1) * p_sel[:, None]
    e_global = g_idx[:, None] * EXPERTS_PER_GROUP + top_i
    gate = jnp.sum(jax.nn.one_hot(e_global, N_EXPERTS, dtype=jnp.float32) * wts[..., None], axis=1)
    y = jnp.zeros_like(xt)
    for e in range(N_EXPERTS):
        hdn = jax.nn.silu(xt @ w_gate[e]) * (xt @ w_up[e])
        y = y + gate[:, e:e + 1].astype(xt.dtype) * (hdn @ w_down[e])
    return y.reshape(B, S, D)


def setup_inputs(seed: int = 0) -> dict:
    key = jax.random.key(seed)
    ks = iter(jax.random.split(key, 48))
    f32 = jnp.float32

    def nrm(shape, scale):
        return jax.random.normal(next(ks), shape, f32) * scale

    def gain(shape):
        return 1.0 + 0.02 * jax.random.normal(next(ks), shape, f32)

    L = DEPTH
    D = D_MODEL
    inputs = {}
    inputs['x'] = nrm((BATCH, SEQ, D), 1.0)
    inputs['mem'] = nrm((BATCH, MEM_LEN, D), 1.0)
    inputs['positions'] = (jnp.arange(SEQ, dtype=jnp.int32)[None, :]
                           + jax.random.randint(next(ks), (BATCH, 1), 0, 1024, dtype=jnp.int32))
    inputs['attn_norm'] = gain((L, D))
    inputs['w_in'] = nrm((L, D, D_IN), D ** -0.5)
    inputs['da_q_norm'] = gain((L, 2, DA_HEAD_DIM))
    inputs['da_k_norm'] = gain((L, 2, DA_HEAD_DIM))
    inputs['da_lambda_q1'] = nrm((L, DA_HEAD_DIM), 0.1)
    inputs['da_lambda_k1'] = nrm((L, DA_HEAD_DIM), 0.1)
    inputs['da_lambda_q2'] = nrm((L, DA_HEAD_DIM), 0.1)
    inputs['da_lambda_k2'] = nrm((L, DA_HEAD_DIM), 0.1)
    inputs['da_subln'] = gain((L, DA_V_DIM))
    inputs['rw_mu'] = jax.random.uniform(next(ks), (L, RW_COLS), f32)
    inputs['rw_w0'] = jax.random.uniform(next(ks), (L, RW_WIDTH), f32, minval=-6.0, maxval=-0.5)
    inputs['rw_w2'] = nrm((L, RW_DECAY_LORA, RW_WIDTH), 0.1 * RW_DECAY_LORA ** -0.5)
    inputs['rw_a0'] = nrm((L, RW_WIDTH), 0.1)
    inputs['rw_a2'] = nrm((L, RW_AAA_LORA, RW_WIDTH), 0.1 * RW_AAA_LORA ** -0.5)
    inputs['rw_g2'] = nrm((L, RW_GATE_LORA, RW_WIDTH), RW_GATE_LORA ** -0.5)
    inputs['rw_k_k'] = 0.85 + 0.02 * jax.random.normal(next(ks), (L, RW_WIDTH), f32)
    inputs['rw_k_a'] = gain((L, RW_WIDTH))
    inputs['rw_r_k'] = nrm((L, RW_HEADS, RW_HEAD_DIM), 0.1)
    inputs['rw_gn_w'] = gain((L, RW_WIDTH))
    inputs['rw_gn_b'] = nrm((L, RW_WIDTH), 0.01)
    inputs['mem_norm'] = gain((L, D))
    inputs['w_mem_kv'] = nrm((L, D, 2 * MEM_WIDTH), D ** -0.5)
    inputs['mem_q_norm'] = gain((L, MEM_HEAD_DIM))
    inputs['mem_k_norm'] = gain((L, MEM_HEAD_DIM))
    inputs['w_out'] = nrm((L, D_MIX, D), D_MIX ** -0.5)
    inputs['ffn_norm'] = gain((L, D))
    inputs['w_group_router'] = nrm((L, D, N_GROUPS), D ** -0.5)
    inputs['b_group_router'] = nrm((L, N_GROUPS), 0.01)
    inputs['w_expert_router'] = nrm((L, D, N_EXPERTS), D ** -0.5)
    inputs['b_expert_router'] = nrm((L, N_EXPERTS), 0.01)
    inputs['w_e_gate'] = nrm((L, N_EXPERTS, D, D_EXPERT), D ** -0.5)
    inputs['w_e_up'] = nrm((L, N_EXPERTS, D, D_EXPERT), D ** -0.5)
    inputs['w_e_down'] = nrm((L, N_EXPERTS, D_EXPERT, D), D_EXPERT ** -0.5)
    return inputs


def reference(x, mem, positions, attn_norm, w_in, da_q_norm, da_k_norm, da_lambda_q1, da_lambda_k1,
              da_lambda_q2, da_lambda_k2, da_subln, rw_mu, rw_w0, rw_w2, rw_a0, rw_a2, rw_g2, rw_k_k,
              rw_k_a, rw_r_k, rw_gn_w, rw_gn_b, mem_norm, w_mem_kv, mem_q_norm, mem_k_norm, w_out,
              ffn_norm, w_group_router, b_group_router, w_expert_router, b_expert_router,
              w_e_gate, w_e_up, w_e_down):
    c1 = DA_QK_WIDTH
    c2 = 2 * DA_QK_WIDTH
    c3 = c2 + DA_WIDTH
    c4 = c3 + RW_COLS
    for l in range(DEPTH):
        h = rms_norm(x, attn_norm[l])
        proj = h @ w_in[l]
        q_da, k_da, v_da, rw_cols, q_mem = jnp.split(proj, [c1, c2, c3, c4], axis=-1)
        lam_init = 0.8 - 0.6 * math.exp(-0.3 * l)
        lam = (jnp.exp(jnp.sum(da_lambda_q1[l].astype(jnp.float32) * da_lambda_k1[l].astype(jnp.float32)))
               - jnp.exp(jnp.sum(da_lambda_q2[l].astype(jnp.float32) * da_lambda_k2[l].astype(jnp.float32)))
               + lam_init)
        o_da = diff_attention(q_da, k_da, v_da, positions, da_q_norm[l], da_k_norm[l], lam, lam_init, da_subln[l])
        o_rw = rwkv7_time_mix(rw_cols, rw_mu[l], rw_w0[l], rw_w2[l], rw_a0[l], rw_a2[l], rw_g2[l],
                              rw_k_k[l], rw_k_a[l], rw_r_k[l], rw_gn_w[l], rw_gn_b[l])
        mem_n = rms_norm(mem, mem_norm[l])
        o_mem = memory_attention(q_mem, mem_n, w_mem_kv[l], mem_q_norm[l], mem_k_norm[l])
        mixed = jnp.concatenate([o_da, o_rw.astype(o_da.dtype), o_mem], axis=-1)
        x = x + mixed @ w_out[l]
        xn = rms_norm(x, ffn_norm[l])
        x = x + hierarchical_moe(xn, w_group_router[l], b_group_router[l], w_expert_router[l],
                                 b_expert_router[l], w_e_gate[l], w_e_up[l], w_e_down[l])
    return x
```

```python
import math
import numpy as np
import ml_dtypes
from contextlib import ExitStack
import concourse.bass as bass
import concourse.mybir as mybir
from concourse.bass_utils import run_bass_kernel_spmd

F32 = mybir.dt.float32
BF16 = mybir.dt.bfloat16
I32 = mybir.dt.int32
AF = mybir.ActivationFunctionType
ALU = mybir.AluOpType
AX = mybir.AxisListType

D = 1024
S = 8192
MEM = 256
NE = 16
DE = 512
EPS = 1e-6
GN_EPS = 64e-5
LAM_INIT = 0.8 - 0.6 * math.exp(-0.3 * 0)


class Reg:
    __slots__ = ("name", "w", "r")

    def __init__(self, name=""):
        self.name = name
        self.w = None
        self.r = []


class KB:
    NDMA = 32

    def __init__(self, nc, es):
        self.nc = nc
        self.es = es
        self.E = dict(pe=nc.tensor, act=nc.scalar, dve=nc.vector, pool=nc.gpsimd, sp=nc.sync)
        self.sem = {}
        self.cnt = {}
        for e in self.E:
            self.sem[e] = es.enter_context(nc.semaphore("s_" + e))
            self.cnt[e] = 0
        self.dsem = [es.enter_context(nc.semaphore("d%d" % i)) for i in range(self.NDMA)]
        self.dcnt = [0] * self.NDMA
        self.dnext = 0
        self.seen = {e: {} for e in self.E}
        self.nalloc = 0

    def sb(self, es, shape, dt, name=None):
        self.nalloc += 1
        return es.enter_context(self.nc.sbuf_tensor("%s_%d" % (name or "t", self.nalloc), list(shape), dt))

    def ps(self, es, shape, dt=F32, name=None):
        self.nalloc += 1
        return es.enter_context(self.nc.psum_tensor("%s_%d" % (name or "p", self.nalloc), list(shape), dt))

    def _semof(self, key):
        return self.sem[key] if isinstance(key, str) else self.dsem[key]

    def _wait(self, e, key, val):
        if key == e:
            if e == "pe":
                return
            if val <= self.cnt[e] - 3:
                return
        s = self.seen[e]
        if s.get(key, 0) >= val:
            return
        self.E[e].wait_ge(self._semof(key), val)
        s[key] = val

    def _deps(self, e, reads, writes):
        for r in reads:
            if r.w is not None:
                self._wait(e, *r.w)
        for r in writes:
            if r.w is not None:
                self._wait(e, *r.w)
            for rd in r.r:
                self._wait(e, *rd)

    def _record(self, tag, reads, writes):
        for r in reads:
            r.r.append(tag)
            if len(r.r) > 48:
                best = {}
                for k, v in r.r:
                    if best.get(k, 0) < v:
                        best[k] = v
                r.r = list(best.items())
        for r in writes:
            r.w = tag
            r.r = []

    def op(self, e, fn, reads=(), writes=()):
        self._deps(e, reads, writes)
        ins = fn(self.E[e])
        self.cnt[e] += 1
        ins.then_inc(self.sem[e], 1)
        self._record((e, self.cnt[e]), reads, writes)
        return ins

    def dma(self, q, out, in_, reads=(), writes=(), **kw):
        i = self.dnext
        self.dnext = (self.dnext + 1) % self.NDMA
        if self.dcnt[i] > 0:
            self._wait(q, i, self.dcnt[i])
        self._deps(q, reads, writes)
        ins = self.E[q].dma_start(out=out, in_=in_, **kw)
        self.dcnt[i] += 16
        ins.then_inc(self.dsem[i], 16)
        self._record((i, self.dcnt[i]), reads, writes)
        return ins

    def barrier(self):
        for e in self.E:
            for f in self.E:
                if f != e and self.cnt[f] > 0:
                    self._wait(e, f, self.cnt[f])
            for i in range(self.NDMA):
                if self.dcnt[i] > 0:
                    self._wait(e, i, self.dcnt[i])


class Cfg:
    def __init__(self, HD=4, HR=4, HM=4, TT=8192, debug=False):
        self.HD, self.HR, self.HM, self.TT, self.debug = HD, HR, HM, TT, debug
        self.NQK = 2 * HD
        self.NMQ = HM // 2
        self.NRW = (3 * HR * 64) // 128 + 2
        self.NF = self.NQK + self.NMQ + self.NRW
        self.VOFF = self.NF * 128
        self.NCOL = self.VOFF + HD * 128
        self.do_da = True
        self.do_mem = True
        self.do_tail = True
        self.rw_zero = False
        self.do_rw = True
        self.tail_tok0 = 0
        self.mem_stage = 1
        self.OFF_RW = HD * 128
        self.OFF_MEM = HD * 128 + HR * 64


def build(cfg):
    nc = bass.Bass("TRN2", target_bir_lowering=False)
    HD, HR, HM = cfg.HD, cfg.HR, cfg.HM
    skind = "ExternalOutput" if cfg.debug else "Internal"

    def din(name, shape, dt=F32):
        return nc.dram_tensor(name, list(shape), dt, kind="ExternalInput").ap()

    def dscr(name, shape, dt):
        return nc.dram_tensor(name, list(shape), dt, kind=skind).ap()

    x_d = din("x", [S, D])
    pos_d = din("pos", [1, S], I32)
    win_d = din("win", [D, cfg.NCOL])
    anorm_d = din("anorm", [128, 8])
    qkg_d = din("qkg", [128, cfg.NQK + cfg.NMQ])
    slopes_d = din("slopes", [128, HD])
    ident_d = din("ident", [128, 128])
    blk_d = din("blk64", [128, 128])
    qkT_d = dscr("qkT", [HD, 2, 2, 68, S], BF16)
    va_d = dscr("vaug", [HD, S, 130], BF16)
    mqT_d = dscr("mqT", [HM * 64, S], BF16)
    rwT_d = dscr("rwT", [cfg.NRW * 128, S], F32)
    mixT_d = dscr("mixT", [D, S], BF16)
    tri_d = din("tri", [128, 128])
    lamv_d = din("lamv", [4, 64])
    mem_d = din("mem", [MEM, D])
    wkv_d = din("wkv", [D, 2 * HM * 64])
    mnorm_d = din("mnorm", [128, 8])
    mkg_d = din("mkg", [128, 1])
    wout_d = din("wout", [D, D])
    rwp_d = din("rwp", [64, HR, 10])
    lmu_d = din("lmu", [128, 3])
    w2_d = din("w2", [64, HR * 64])
    a2_d = din("a2", [64, HR * 64])
    g2_d = din("g2", [128, HR * 64])
    mask4_d = din("mask4", [128, 512])
    masksl_d = din("masksl", [128, 128])
    rmask_d = din("rmask", [64, 512])
    wosc_d = din("wosc", [128, 8])
    fnorm_d = din("fnorm", [128, 8])
    rbias_d = din("rbias", [20])
    wr_d = din("wr", [D, 20])
    wge_d = din("wge", [NE, D, DE])
    wue_d = din("wue", [NE, D, DE])
    wde_d = din("wde", [NE, DE, D])
    xo_d = din("xo", [cfg.TT, D])
    out_d = nc.dram_tensor("out", [cfg.TT, D], F32, kind="ExternalOutput").ap()

    with ExitStack() as es0:
        kb = KB(nc, es0)
        ident = kb.sb(es0, [128, 128], BF16, "ident")
        blk = kb.sb(es0, [128, 128], BF16, "blk")
        r_const = Reg("const")
        kb.dma("pool", ident[:], ident_d, writes=[r_const])
        kb.dma("pool", blk[:], blk_d, writes=[r_const])

        with ExitStack() as es:
            wbf = kb.sb(es, [128, 8, cfg.NCOL], BF16, "wbf")
            r_w = Reg("wbf")
            wst = [kb.sb(es, [128, cfg.NCOL], F32, "wst") for _ in range(2)]
            r_wst = [Reg(), Reg()]
            anorm = kb.sb(es, [128, 8], F32, "anorm")
            qkg = kb.sb(es, [128, cfg.NQK + cfg.NMQ], F32, "qkg")
            r_par = Reg("par")
            kb.dma("sp", anorm[:], anorm_d, writes=[r_par])
            kb.dma("sp", qkg[:], qkg_d, writes=[r_par])
            for kc in range(8):
                b = kc % 2
                kb.dma("sp", wst[b][:], win_d[kc * 128:(kc + 1) * 128, :], writes=[r_wst[b]])
                eng = "dve" if kc % 2 == 0 else "pool"
                kb.op(eng, lambda e, kc=kc, b=b: e.tensor_scalar(
                    out=wbf[:, kc, :], in0=wst[b][:], scalar1=anorm[:, kc:kc + 1], scalar2=None, op0=ALU.mult),
                    reads=[r_wst[b], r_par], writes=[r_w])

            posi = kb.sb(es, [128, 64], I32, "posi")
            posf = kb.sb(es, [128, 64], F32, "posf")
            hib = kb.sb(es, [128, 64], BF16, "hib")
            hif = kb.sb(es, [128, 64], F32, "hif")
            lof = kb.sb(es, [128, 64], F32, "lof")
            kaug = kb.sb(es, [128, 4, 64], BF16, "kaug")
            qaug = kb.sb(es, [128, HD, 4, 64], BF16, "qaug")
            slp = kb.sb(es, [128, HD], F32, "slp")
            nslp = kb.sb(es, [128, HD], F32, "nslp")
            r_pos = Reg("pos")
            r_aug = Reg("aug")
            kb.dma("sp", posi[:], pos_d.rearrange("o (p j) -> (o p) j", j=64), writes=[r_pos])
            kb.dma("sp", slp[:], slopes_d, writes=[r_pos])
            kb.op("dve", lambda e: e.tensor_copy(out=posf[:], in_=posi[:]), reads=[r_pos], writes=[r_pos])
            kb.op("dve", lambda e: e.tensor_copy(out=hib[:], in_=posf[:]), reads=[r_pos], writes=[r_pos])
            kb.op("dve", lambda e: e.tensor_copy(out=hif[:], in_=hib[:]), reads=[r_pos], writes=[r_pos])
            kb.op("dve", lambda e: e.tensor_sub(out=lof[:], in0=posf[:], in1=hif[:]), reads=[r_pos], writes=[r_pos])
            kb.op("dve", lambda e: e.tensor_scalar(out=nslp[:], in0=slp[:], scalar1=-1.0, scalar2=None, op0=ALU.mult),
                  reads=[r_pos], writes=[r_pos])
            kb.op("pool", lambda e: e.memset(kaug[:, 0:2, :], 1.0), writes=[r_aug])
            kb.op("dve", lambda e: e.tensor_copy(out=kaug[:, 2, :], in_=hif[:]), reads=[r_pos], writes=[r_aug])
            kb.op("dve", lambda e: e.tensor_copy(out=kaug[:, 3, :], in_=lof[:]), reads=[r_pos], writes=[r_aug])
            kb.op("pool", lambda e: e.memset(qaug[:, :, 2:4, :], 1.0), writes=[r_aug])
            for h in range(HD):
                kb.op("dve", lambda e, h=h: e.tensor_scalar(out=qaug[:, h, 0, :], in0=hif[:], scalar1=nslp[:, h:h + 1],
                                                        scalar2=None, op0=ALU.mult), reads=[r_pos], writes=[r_aug])
                kb.op("dve", lambda e, h=h: e.tensor_scalar(out=qaug[:, h, 1, :], in0=lof[:], scalar1=nslp[:, h:h + 1],
                                                        scalar2=None, op0=ALU.mult), reads=[r_pos], writes=[r_aug])
                kb.op("dve", lambda e, h=h: e.tensor_scalar(out=qaug[:, h, 2:4, :], in0=qaug[:, h, 2:4, :],
                                                        scalar1=slp[:, h:h + 1], scalar2=None, op0=ALU.mult),
                      reads=[r_pos, r_aug], writes=[r_aug])
            r_qk_d = Reg("qkT_d")
            for h in range(HD):
                for m in range(2):
                    kb.dma("sp", qkT_d[h, 0, m, 64:68, :].rearrange("r (p j) -> p r j", j=64), qaug[:, h, :, :],
                           reads=[r_aug], writes=[r_qk_d])
                    kb.dma("sp", qkT_d[h, 1, m, 64:68, :].rearrange("r (p j) -> p r j", j=64), kaug[:, :, :],
                           reads=[r_aug], writes=[r_qk_d])

            xt = [kb.sb(es, [128, 4, D], F32, "xt") for _ in range(2)]
            r_xt = [Reg(), Reg()]
            xs = kb.sb(es, [128, 4, D], BF16, "xs")
            r_xs = [Reg() for _ in range(4)]
            junk = kb.sb(es, [128, D], BF16, "junk")
            r_junk = Reg()
            ss = kb.sb(es, [128, 4], F32, "ss")
            rstd = kb.sb(es, [128, 4], F32, "rstd")
            r_ss = Reg()
            r_rstd = Reg()
            hT = [kb.sb(es, [128, 8, 512], BF16, "hT") for _ in range(2)]
            r_hT = [Reg(), Reg()]
            ptr = [kb.ps(es, [128, 8, 128], BF16, "ptr") for _ in range(2)]
            r_ptr = [Reg(), Reg()]
            pf = [kb.ps(es, [128, 512], F32, "pf") for _ in range(2)]
            r_pf = [Reg(), Reg()]
            pm = [kb.ps(es, [128, 512], F32, "pm") for _ in range(2)]
            r_pm = [Reg(), Reg()]
            pv = [kb.ps(es, [128, HD * 128], F32, "pv") for _ in range(2)]
            r_pv = [Reg(), Reg()]
            sq = [kb.sb(es, [128, 512], BF16, "sq") for _ in range(2)]
            r_sq = [Reg(), Reg()]
            rs = [kb.sb(es, [128, 512], F32, "rs") for _ in range(2)]
            r_rs = [Reg(), Reg()]
            qst = [kb.sb(es, [128, 512], BF16, "qst") for _ in range(3)]
            r_qst = [Reg() for _ in range(3)]
            fst = [kb.sb(es, [128, 512], F32, "fst") for _ in range(3)]
            r_fst = [Reg() for _ in range(3)]
            vst = [kb.sb(es, [128, HD, 130], BF16, "vst") for _ in range(2)]
            r_vst = [Reg(), Reg()]
            for b in range(2):
                kb.op("pool", lambda e, b=b: e.memset(vst[b][:, :, 128:130], 1.0), writes=[r_vst[b]])
            r_out = Reg("scratch_out")
            NT = S // 512
            x_v = x_d.rearrange("(t s p) d -> t p s d", s=4, p=128)
            va_v = va_d.rearrange("h (t s p) e -> t s p h e", s=4, p=128)
            nq = 0
            nf = 0
            nv = 0
            kb.dma("sp", xt[0][:], x_v[0], writes=[r_xt[0]])
            for t in range(NT):
                tb = t % 2
                if t + 1 < NT:
                    kb.dma("sp", xt[1 - tb][:], x_v[t + 1], writes=[r_xt[1 - tb]])
                for s in range(4):
                    kb.op("act", lambda e, s=s: e.activation(out=junk[:], in_=xt[tb][:, s, :], func=AF.Square,
                                                            accum_out=ss[:, s:s + 1]),
                          reads=[r_xt[tb]], writes=[r_junk, r_ss])
                kb.op("act", lambda e: e.activation(out=rstd[:], in_=ss[:], func=AF.Sqrt, bias=EPS, scale=1.0 / D),
                      reads=[r_ss], writes=[r_rstd])
                kb.op("dve", lambda e: e.reciprocal(out=rstd[:], in_=rstd[:]), reads=[r_rstd], writes=[r_rstd])
                for s in range(4):
                    kb.op("dve", lambda e, s=s: e.tensor_scalar(out=xs[:, s, :], in0=xt[tb][:, s, :],
                                                            scalar1=rstd[:, s:s + 1], scalar2=None, op0=ALU.mult),
                          reads=[r_xt[tb], r_rstd], writes=[r_xs[s]])
                for s in range(4):
                    pb = s % 2
                    for kc in range(8):
                        kb.op("pe", lambda e, s=s, kc=kc, pb=pb: e.transpose(
                            out=ptr[pb][:, kc, :], in_=xs[:, s, kc * 128:(kc + 1) * 128], identity=ident[:]),
                            reads=[r_xs[s], r_const], writes=[r_ptr[pb]])
                    kb.op("act", lambda e, s=s, pb=pb: e.activation(out=hT[tb][:, :, s * 128:(s + 1) * 128],
                                                                  in_=ptr[pb][:], func=AF.Copy),
                          reads=[r_ptr[pb]], writes=[r_hT[tb]])
                for c in range(cfg.NF):
                    fb = c % 2
                    for kc in range(8):
                        kb.op("pe", lambda e, c=c, kc=kc, fb=fb: e.matmul(
                            pf[fb][:], lhsT=wbf[:, kc, c * 128:(c + 1) * 128], rhs=hT[tb][:, kc, :],
                            start=(kc == 0), stop=(kc == 7)), reads=[r_w, r_hT[tb]], writes=[r_pf[fb]])
                    if c < cfg.NQK + cfg.NMQ:
                        kb.op("act", lambda e, fb=fb: e.activation(out=sq[fb][:], in_=pf[fb][:], func=AF.Square),
                              reads=[r_pf[fb]], writes=[r_sq[fb]])
                        kb.op("pe", lambda e, fb=fb: e.matmul(pm[fb][:], lhsT=blk[:], rhs=sq[fb][:], start=True, stop=True),
                              reads=[r_sq[fb], r_const], writes=[r_pm[fb]])
                        qsc = 64.0 if (c >= cfg.NQK or c % 2 == 0) else 1.0
                        kb.op("act", lambda e, fb=fb, qsc=qsc: e.activation(out=rs[fb][:], in_=pm[fb][:], func=AF.Sqrt,
                                                                           bias=EPS * qsc, scale=qsc),
                              reads=[r_pm[fb]], writes=[r_rs[fb]])
                        kb.op("dve", lambda e, fb=fb: e.reciprocal(out=rs[fb][:], in_=rs[fb][:]), reads=[r_rs[fb]], writes=[r_rs[fb]])
                        qb = nq % 3
                        nq += 1
                        kb.op("dve", lambda e, fb=fb, c=c, qb=qb: e.scalar_tensor_tensor(
                            out=qst[qb][:], in0=pf[fb][:], scalar=qkg[:, c:c + 1], in1=rs[fb][:], op0=ALU.mult, op1=ALU.mult),
                            reads=[r_pf[fb], r_rs[fb], r_par], writes=[r_qst[qb]])
                        if c < cfg.NQK:
                            h, qk = c // 2, c % 2
                            for m in range(2):
                                kb.dma("sp", qkT_d[h, qk, m, 0:64, t * 512:(t + 1) * 512], qst[qb][m * 64:(m + 1) * 64, :],
                                       reads=[r_qst[qb]], writes=[r_out])
                        else:
                            cm = c - cfg.NQK
                            kb.dma("sp", mqT_d[cm * 128:(cm + 1) * 128, t * 512:(t + 1) * 512], qst[qb][:],
                                   reads=[r_qst[qb]], writes=[r_out])
                    else:
                        cr = c - cfg.NQK - cfg.NMQ
                        ob = nf % 3
                        nf += 1
                        kb.op("act", lambda e, fb=fb, ob=ob: e.activation(out=fst[ob][:], in_=pf[fb][:], func=AF.Copy),
                              reads=[r_pf[fb]], writes=[r_fst[ob]])
                        kb.dma("sp", rwT_d[cr * 128:(cr + 1) * 128, t * 512:(t + 1) * 512], fst[ob][:],
                               reads=[r_fst[ob]], writes=[r_out])
                for s in range(4):
                    vb = nv % 2
                    nv += 1
                    for kc in range(8):
                        kb.op("pe", lambda e, s=s, kc=kc, vb=vb: e.matmul(
                            pv[vb][:], lhsT=hT[tb][:, kc, s * 128:(s + 1) * 128], rhs=wbf[:, kc, cfg.VOFF:cfg.VOFF + HD * 128],
                            start=(kc == 0), stop=(kc == 7)), reads=[r_w, r_hT[tb]], writes=[r_pv[vb]])
                    kb.op("dve", lambda e, vb=vb: e.tensor_copy(
                        out=vst[vb][:, :, 0:128], in_=pv[vb][:].rearrange("p (h e) -> p h e", h=HD)),
                        reads=[r_pv[vb]], writes=[r_vst[vb]])
                    kb.dma("sp", va_v[t, s], vst[vb][:], reads=[r_vst[vb]], writes=[r_out])
            kb.barrier()

        if cfg.do_da:
          with ExitStack() as es:
            KT = [[kb.sb(es, [68, S], BF16, "KT") for m in range(2)] for b in range(2)]
            VA = [kb.sb(es, [128, S // 128, 130], BF16, "VA") for b in range(2)]
            r_kv = [Reg(), Reg()]
            QT = [[kb.sb(es, [68, 512], BF16, "QT") for m in range(2)] for b in range(2)]
            r_q = [Reg(), Reg()]
            tri = kb.sb(es, [128, 128], BF16, "tri")
            r_tri = Reg()
            kb.dma("pool", tri[:], tri_d, writes=[r_tri])
            lamv = kb.sb(es, [128, 4, 64], F32, "lamv")
            lamt = kb.sb(es, [128, 2, 64], F32, "lamt")
            lams = kb.sb(es, [128, 4], F32, "lams")
            r_lam = Reg()
            kb.dma("sp", lamv[:], lamv_d.partition_broadcast(128), writes=[r_lam])
            kb.op("dve", lambda e: e.tensor_tensor(out=lamt[:], in0=lamv[:, 0:4:2, :], in1=lamv[:, 1:4:2, :], op=ALU.mult),
                  reads=[r_lam], writes=[r_lam])
            kb.op("dve", lambda e: e.tensor_reduce(out=lams[:, 0:2], in_=lamt[:], axis=AX.X, op=ALU.add),
                  reads=[r_lam], writes=[r_lam])
            kb.op("act", lambda e: e.activation(out=lams[:, 0:2], in_=lams[:, 0:2], func=AF.Exp), reads=[r_lam], writes=[r_lam])
            kb.op("dve", lambda e: e.scalar_tensor_tensor(out=lams[:, 2:3], in0=lams[:, 1:2], scalar=-LAM_INIT, in1=lams[:, 0:1],
                                                         op0=ALU.add, op1=ALU.subtract), reads=[r_lam], writes=[r_lam])
            nlam = lams[:, 2:3]
            ps_s = [kb.ps(es, [128, 512], F32, "ps_s") for _ in range(3)]
            r_s = [Reg() for _ in range(3)]
            po = [[kb.ps(es, [128, 512], F32, "po") for half in range(2)] for m in range(2)]
            r_po = [[Reg(), Reg()], [Reg(), Reg()]]
            ptr2 = kb.ps(es, [128, 4, 256], BF16, "ptr2")
            r_ptr2 = Reg()
            NPB = 4
            P = [kb.sb(es, [128, 512], BF16, "P") for _ in range(NPB)]
            r_P = [Reg() for _ in range(NPB)]
            oc = [kb.sb(es, [128, 4, 130], F32, "oc") for m in range(2)]
            r_oc = [Reg(), Reg()]
            rr = kb.sb(es, [128, 2, 4], F32, "rr")
            r_rr = Reg()
            dd = kb.sb(es, [128, 4, 128], F32, "dd")
            r_dd = Reg()
            aa = kb.sb(es, [128, 4, 128], F32, "aa")
            r_aa = Reg()
            ssd = kb.sb(es, [128, 4], F32, "ssd")
            r_ssd = Reg()
            junk2 = kb.sb(es, [128, 128], BF16, "junk2")
            r_junk2 = Reg()
            dn = kb.sb(es, [128, 4, 128], BF16, "dn")
            r_dn = Reg()
            mst = [kb.sb(es, [128, 512], BF16, "mst") for _ in range(2)]
            r_mst = [Reg(), Reg()]
            r_mix = Reg("mix_d")

            def load_kv(h):
                b = h % 2
                for m in range(2):
                    kb.dma("sp", KT[b][m][:], qkT_d[h, 1, m], writes=[r_kv[b]])
                kb.dma("sp", VA[b][:], va_d[h].rearrange("(t p) e -> p t e", p=128), writes=[r_kv[b]])

            def load_q(h, qi, b):
                for m in range(2):
                    kb.dma("sp", QT[b][m][:], qkT_d[h, 0, m, :, qi * 512:(qi + 1) * 512], writes=[r_q[b]])

            NQT = S // 512
            tiles = [(h, qi) for h in range(HD) for qi in range(NQT)]
            load_kv(0)
            load_q(0, 0, 0)
            items = []
            for ti, (h, qi) in enumerate(tiles):
                for kt in range(4 * (qi + 1)):
                    for m in range(2):
                        items.append((ti, h, qi, kt, m))

            def emit_S(i):
                ti, h, qi, kt, m = items[i]
                j = kt - 4 * qi
                q0 = max(j, 0) * 128
                sbi = i % 3
                kb.op("pe", lambda e: e.matmul(ps_s[sbi][:, q0:512], lhsT=KT[h % 2][m][:, kt * 128:(kt + 1) * 128],
                                              rhs=QT[ti % 2][m][:, q0:512], start=True, stop=True),
                      reads=[r_kv[h % 2], r_q[ti % 2]], writes=[r_s[sbi]])

            def emit_rest(i):
                ti, h, qi, kt, m = items[i]
                j = kt - 4 * qi
                q0 = max(j, 0) * 128
                sbi = i % 3
                pbi = i % NPB
                kb.op("act", lambda e: e.activation(out=P[pbi][:, q0:512], in_=ps_s[sbi][:, q0:512], func=AF.Exp),
                      reads=[r_s[sbi]], writes=[r_P[pbi]])
                if j >= 0:
                    kb.op("pool", lambda e: e.tensor_tensor(out=P[pbi][:, q0:q0 + 128], in0=P[pbi][:, q0:q0 + 128], in1=tri[:],
                                                           op=ALU.mult), reads=[r_P[pbi], r_tri], writes=[r_P[pbi]])
                for qs in range(max(j, 0), 4):
                    kb.op("pe", lambda e, qs=qs: e.matmul(po[m][qs // 2][:, (qs % 2) * 130:(qs % 2) * 130 + 130],
                                                         lhsT=P[pbi][:, qs * 128:(qs + 1) * 128], rhs=VA[h % 2][:, kt, :],
                                                         start=(kt == 0 and qs % 2 == 0), stop=(kt == 4 * qi + qs),
                                                         skip_group_check=True),
                          reads=[r_P[pbi], r_kv[h % 2]], writes=[r_po[m][qs // 2]])

            def finalize(ti, h, qi):
                for m in range(2):
                    for half in range(2):
                        eng = "act" if half == 0 else "dve"
                        src = po[m][half][:, 0:260].rearrange("p (a e) -> p a e", a=2)
                        if eng == "act":
                            kb.op("act", lambda e, m=m, half=half, src=src: e.activation(out=oc[m][:, 2 * half:2 * half + 2, :], in_=src, func=AF.Copy),
                                  reads=[r_po[m][half]], writes=[r_oc[m]])
                        else:
                            kb.op("dve", lambda e, m=m, half=half, src=src: e.tensor_copy(out=oc[m][:, 2 * half:2 * half + 2, :], in_=src),
                                  reads=[r_po[m][half]], writes=[r_oc[m]])
                for m in range(2):
                    kb.op("dve", lambda e, m=m: e.reciprocal(out=rr[:, m, :], in_=oc[m][:, :, 128]), reads=[r_oc[m]], writes=[r_rr])
                kb.op("dve", lambda e: e.tensor_scalar(out=rr[:, 1, :], in0=rr[:, 1, :], scalar1=nlam, scalar2=None, op0=ALU.mult),
                      reads=[r_rr, r_lam], writes=[r_rr])
                for qs in range(4):
                    kb.op("pool", lambda e, qs=qs: e.tensor_scalar(out=aa[:, qs, :], in0=oc[0][:, qs, 0:128], scalar1=rr[:, 0, qs:qs + 1],
                                                                 scalar2=None, op0=ALU.mult), reads=[r_oc[0], r_rr], writes=[r_aa])
                for qs in range(4):
                    kb.op("dve", lambda e, qs=qs: e.scalar_tensor_tensor(out=dd[:, qs, :], in0=oc[1][:, qs, 0:128], scalar=rr[:, 1, qs:qs + 1],
                                                                        in1=aa[:, qs, :], op0=ALU.mult, op1=ALU.add),
                          reads=[r_oc[1], r_rr, r_aa], writes=[r_dd])
                for qs in range(4):
                    kb.op("act", lambda e, qs=qs: e.activation(out=junk2[:], in_=dd[:, qs, :], func=AF.Square, accum_out=ssd[:, qs:qs + 1]),
                          reads=[r_dd], writes=[r_junk2, r_ssd])
                kb.op("act", lambda e: e.activation(out=ssd[:], in_=ssd[:], func=AF.Sqrt, bias=EPS / (1.0 - LAM_INIT) ** 2,
                                                    scale=1.0 / (128 * (1.0 - LAM_INIT) ** 2)), reads=[r_ssd], writes=[r_ssd])
                kb.op("dve", lambda e: e.reciprocal(out=ssd[:], in_=ssd[:]), reads=[r_ssd], writes=[r_ssd])
                for qs in range(4):
                    kb.op("pool", lambda e, qs=qs: e.tensor_scalar(out=dn[:, qs, :], in0=dd[:, qs, :], scalar1=ssd[:, qs:qs + 1],
                                                                 scalar2=None, op0=ALU.mult), reads=[r_dd, r_ssd], writes=[r_dn])
                for qs in range(4):
                    kb.op("pe", lambda e, qs=qs: e.transpose(out=ptr2[:, qs, 0:128], in_=dn[:, qs, :], identity=ident[:]),
                          reads=[r_dn, r_const], writes=[r_ptr2])
                mb = ti % 2
                kb.op("act", lambda e: e.activation(out=mst[mb][:].rearrange("p (a q) -> p a q", a=4), in_=ptr2[:, :, 0:128], func=AF.Copy),
                      reads=[r_ptr2], writes=[r_mst[mb]])
                kb.dma("sp", mixT_d[h * 128:(h + 1) * 128, qi * 512:(qi + 1) * 512], mst[mb][:], reads=[r_mst[mb]], writes=[r_mix])

            LOOK = 2
            n = len(items)
            for i in range(min(LOOK, n)):
                emit_S(i)
            cur = 0
            for i in range(n):
                ti, h, qi, kt, m = items[i]
                if kt == 0 and m == 0:
                    if ti + 1 < len(tiles):
                        nh, nqi = tiles[ti + 1]
                        load_q(nh, nqi, (ti + 1) % 2)
                    if qi == 0 and h + 1 < HD:
                        load_kv(h + 1)
                if i + LOOK < n:
                    emit_S(i + LOOK)
                emit_rest(i)
                if kt == 4 * (qi + 1) - 1 and m == 1:
                    finalize(ti, h, qi)
            kb.barrier()

        if cfg.do_mem:
          with ExitStack() as es:
            NKV = 2 * HM * 64
            NPAIR = HM // 2
            wkv = kb.sb(es, [128, 8, NKV], BF16, "wkv")
            r_wkv = Reg()
            wks = [kb.sb(es, [128, NKV], F32, "wks") for _ in range(2)]
            r_wks = [Reg(), Reg()]
            mnorm = kb.sb(es, [128, 8], F32, "mnorm")
            mkg = kb.sb(es, [128, 1], F32, "mkg")
            r_mp = Reg()
            kb.dma("sp", mnorm[:], mnorm_d, writes=[r_mp])
            kb.dma("sp", mkg[:], mkg_d, writes=[r_mp])
            for kc in range(8):
                b = kc % 2
                kb.dma("sp", wks[b][:], wkv_d[kc * 128:(kc + 1) * 128, :], writes=[r_wks[b]])
                kb.op("dve", lambda e, kc=kc, b=b: e.tensor_scalar(out=wkv[:, kc, :], in0=wks[b][:], scalar1=mnorm[:, kc:kc + 1],
                                                              scalar2=None, op0=ALU.mult), reads=[r_wks[b], r_mp], writes=[r_wkv])
            mt = kb.sb(es, [128, 2, D], F32, "mt")
            mtb = kb.sb(es, [128, 2, D], BF16, "mtb")
            r_mt = Reg()
            r_mtb = Reg()
            mjunk = kb.sb(es, [128, D], BF16, "mjunk")
            mss = kb.sb(es, [128, 2], F32, "mss")
            r_mss = Reg()
            memT = kb.sb(es, [128, 8, MEM], BF16, "memT")
            r_memT = Reg()
            ptm = kb.ps(es, [128, 8, 128], BF16, "ptm")
            r_ptm = Reg()
            kb.dma("sp", mt[:], mem_d.rearrange("(s p) d -> p s d", p=128), writes=[r_mt])
            for s_ in range(2):
                kb.op("act", lambda e, s_=s_: e.activation(out=mjunk[:], in_=mt[:, s_, :], func=AF.Square, accum_out=mss[:, s_:s_ + 1]),
                      reads=[r_mt], writes=[r_mss])
            kb.op("act", lambda e: e.activation(out=mss[:], in_=mss[:], func=AF.Sqrt, bias=EPS, scale=1.0 / D), reads=[r_mss], writes=[r_mss])
            kb.op("dve", lambda e: e.reciprocal(out=mss[:], in_=mss[:]), reads=[r_mss], writes=[r_mss])
            for s_ in range(2):
                kb.op("dve", lambda e, s_=s_: e.tensor_scalar(out=mtb[:, s_, :], in0=mt[:, s_, :], scalar1=mss[:, s_:s_ + 1], scalar2=None,
                                                            op0=ALU.mult), reads=[r_mt, r_mss], writes=[r_mtb])
            for s_ in range(2):
                for kc in range(8):
                    kb.op("pe", lambda e, s_=s_, kc=kc: e.transpose(out=ptm[:, kc, :], in_=mtb[:, s_, kc * 128:(kc + 1) * 128], identity=ident[:]),
                          reads=[r_mtb, r_const], writes=[r_ptm])
                kb.op("act", lambda e, s_=s_: e.activation(out=memT[:, :, s_ * 128:(s_ + 1) * 128], in_=ptm[:], func=AF.Copy),
                      reads=[r_ptm], writes=[r_memT])
            mkT = [kb.sb(es, [128, MEM], BF16, "mkT") for _ in range(NPAIR)]
            r_mkT = Reg()
            mva = kb.sb(es, [128, 2, HM, 66], BF16, "mva")
            r_mva = Reg()
            kb.op("pool", lambda e: e.memset(mva[:, :, :, 64:66], 1.0), writes=[r_mva])
            pk = kb.ps(es, [128, 512], F32, "pk")
            r_pk = Reg()
            pk2 = kb.ps(es, [128, 512], F32, "pk2")
            r_pk2 = Reg()
            msq = kb.sb(es, [128, MEM], BF16, "msq")
            mrs = kb.sb(es, [128, MEM], F32, "mrs")
            r_msq = Reg()
            r_mrs = Reg()
            for c in range(NPAIR):
                for kc in range(8):
                    kb.op("pe", lambda e, c=c, kc=kc: e.matmul(pk[:, 0:MEM], lhsT=wkv[:, kc, c * 128:(c + 1) * 128], rhs=memT[:, kc, :],
                                                            start=(kc == 0), stop=(kc == 7)), reads=[r_wkv, r_memT], writes=[r_pk])
                kb.op("act", lambda e: e.activation(out=msq[:], in_=pk[:, 0:MEM], func=AF.Square), reads=[r_pk], writes=[r_msq])
                kb.op("pe", lambda e: e.matmul(pk2[:, 0:MEM], lhsT=blk[:], rhs=msq[:], start=True, stop=True), reads=[r_msq, r_const], writes=[r_pk2])
                kb.op("act", lambda e: e.activation(out=mrs[:], in_=pk2[:, 0:MEM], func=AF.Sqrt, bias=EPS, scale=1.0), reads=[r_pk2], writes=[r_mrs])
                kb.op("dve", lambda e: e.reciprocal(out=mrs[:], in_=mrs[:]), reads=[r_mrs], writes=[r_mrs])
                kb.op("dve", lambda e, c=c: e.scalar_tensor_tensor(out=mkT[c][:], in0=pk[:, 0:MEM], scalar=mkg[:, 0:1], in1=mrs[:],
                                                                 op0=ALU.mult, op1=ALU.mult), reads=[r_pk, r_mrs, r_mp], writes=[r_mkT])
            for s_ in range(2):
                for kc in range(8):
                    kb.op("pe", lambda e, s_=s_, kc=kc: e.matmul(pk[:, 0:HM * 64], lhsT=memT[:, kc, s_ * 128:(s_ + 1) * 128],
                                                              rhs=wkv[:, kc, HM * 64:2 * HM * 64], start=(kc == 0), stop=(kc == 7)),
                          reads=[r_wkv, r_memT], writes=[r_pk])
                kb.op("dve", lambda e, s_=s_: e.tensor_copy(out=mva[:, s_, :, 0:64], in_=pk[:, 0:HM * 64].rearrange("p (h d) -> p h d", h=HM)),
                      reads=[r_pk], writes=[r_mva])
            mq = [[kb.sb(es, [128, 512], BF16, "mq") for h in range(HM)] for b in range(2)]
            r_mq = [Reg(), Reg()]
            for b in range(2):
                for h in range(HM):
                    kb.op("pool", lambda e, b=b, h=h: e.memset(mq[b][h][:], 0.0), writes=[r_mq[b]])
            ps_m = [kb.ps(es, [128, 512], F32, "ps_m") for _ in range(2)]
            r_psm = [Reg(), Reg()]
            pom = [kb.ps(es, [128, 512], F32, "pom") for _ in range(2)]
            r_pom = [Reg(), Reg()]
            Pm = [kb.sb(es, [128, 512], BF16, "Pm") for _ in range(3)]
            r_Pm = [Reg() for _ in range(3)]
            rrm = kb.sb(es, [128, 4], F32, "rrm")
            r_rrm = Reg()
            om = kb.sb(es, [128, 4, HM * 64], BF16, "om")
            r_om = Reg()
            mstm = [kb.sb(es, [128, NPAIR, 512], BF16, "mstm") for _ in range(2)]
            r_mstm = [Reg(), Reg()]
            r_mixm = Reg()
            NQT = S // 512

            def load_mq(qi):
                for h in range(HM):
                    hh = h % 2
                    kb.dma("sp", mq[qi % 2][h][hh * 64:(hh + 1) * 64, :], mqT_d[h * 64:(h + 1) * 64, qi * 512:(qi + 1) * 512],
                           writes=[r_mq[qi % 2]])

            load_mq(0)
            cnt = 0
            for qi in range(NQT if cfg.mem_stage > 0 else 0):
                if qi + 1 < NQT:
                    load_mq(qi + 1)
                for h in range(HM):
                    c, hh = h // 2, h % 2
                    pb = h % 2
                    for kt in range(2):
                        sbi = cnt % 2
                        pbi = cnt % 3
                        cnt += 1
                        kb.op("pe", lambda e: e.matmul(ps_m[sbi][:], lhsT=mkT[c][:, kt * 128:(kt + 1) * 128],
                                                      rhs=mq[qi % 2][h][:, :], start=True, stop=True),
                              reads=[r_mkT, r_mq[qi % 2]], writes=[r_psm[sbi]])
                        kb.op("act", lambda e: e.activation(out=Pm[pbi][:], in_=ps_m[sbi][:], func=AF.Exp), reads=[r_psm[sbi]], writes=[r_Pm[pbi]])
                        for qs in range(4):
                            kb.op("pe", lambda e, qs=qs: e.matmul(pom[pb][:, qs * 66:(qs + 1) * 66], lhsT=Pm[pbi][:, qs * 128:(qs + 1) * 128],
                                                                 rhs=mva[:, kt, h, :], start=(kt == 0 and qs == 0), stop=(kt == 1),
                                                                 skip_group_check=True),
                                  reads=[r_Pm[pbi], r_mva], writes=[r_pom[pb]])
                    pv3 = pom[pb][:, 0:264].rearrange("p (a e) -> p a e", a=4)
                    kb.op("dve", lambda e, pv3=pv3: e.reciprocal(out=rrm[:], in_=pv3[:, :, 64]), reads=[r_pom[pb]], writes=[r_rrm])
                    for qs in range(4):
                        kb.op("dve", lambda e, qs=qs, pv3=pv3, h=h: e.tensor_scalar(out=om[:, qs, h * 64:(h + 1) * 64], in0=pv3[:, qs, 0:64],
                                                                                 scalar1=rrm[:, qs:qs + 1], scalar2=None, op0=ALU.mult),
                              reads=[r_pom[pb], r_rrm], writes=[r_om])
                for c in range(NPAIR):
                    for qs in range(4):
                        kb.op("pe", lambda e, c=c, qs=qs: e.transpose(out=ptm[:, c * 4 + qs, :], in_=om[:, qs, c * 128:(c + 1) * 128], identity=ident[:]),
                              reads=[r_om, r_const], writes=[r_ptm])
                mb = qi % 2
                kb.op("dve", lambda e, mb=mb: e.tensor_copy(out=mstm[mb][:].rearrange("p c (a q) -> p (c a) q", a=4), in_=ptm[:, 0:NPAIR * 4, :]),
                      reads=[r_ptm], writes=[r_mstm[mb]])
                for c in range(NPAIR):
                    kb.dma("sp", mixT_d[cfg.OFF_MEM + c * 128:cfg.OFF_MEM + (c + 1) * 128, qi * 512:(qi + 1) * 512], mstm[mb][:, c, :],
                           reads=[r_mstm[mb]], writes=[r_mixm])
            kb.barrier()

        if cfg.do_rw:
          with ExitStack() as es:
            E05 = math.exp(-0.5)
            R0, K0, V0 = 0, HR * 64, 2 * HR * 64
            LW0 = 3 * HR * 64
            LG0 = LW0 + 128
            rwp = kb.sb(es, [64, HR, 12], F32, "rwp")
            lmu = kb.sb(es, [128, 4], F32, "lmu")
            w2b = kb.sb(es, [64, HR * 64], BF16, "w2b")
            a2b = kb.sb(es, [64, HR * 64], BF16, "a2b")
            g2b = kb.sb(es, [128, HR * 64], BF16, "g2b")
            ones64 = kb.sb(es, [64, 64], F32, "ones64")
            idf = kb.sb(es, [64, 64], F32, "idf")
            mask4 = kb.sb(es, [128, 512], BF16, "mask4")
            masksl = kb.sb(es, [128, 128], BF16, "masksl")
            rmask = kb.sb(es, [64, 512], F32, "rmask")
            r_rc = Reg()
            kb.dma("sp", rwp[:, :, 0:10], rwp_d, writes=[r_rc])
            kb.dma("sp", lmu[:, 0:3], lmu_d, writes=[r_rc])
            kb.dma("pool", w2b[:], w2_d, writes=[r_rc])
            kb.dma("pool", a2b[:], a2_d, writes=[r_rc])
            kb.dma("pool", g2b[:], g2_d, writes=[r_rc])
            kb.dma("sp", idf[:], ident_d[0:64, 0:64], writes=[r_rc])
            kb.dma("pool", mask4[:], mask4_d, writes=[r_rc])
            kb.dma("pool", masksl[:], masksl_d, writes=[r_rc])
            kb.dma("sp", rmask[:], rmask_d, writes=[r_rc])
            kb.op("pool", lambda e: e.memset(ones64[:], 1.0), writes=[r_rc])
            PMU_R, PMU_K, PMU_V, PW0, PA0, PKK, PKA, PRK, PGW, PGB, POMKA = range(11)
            for h in range(HR):
                kb.op("dve", lambda e, h=h: e.tensor_scalar(out=rwp[:, h, POMKA:POMKA + 1], in0=rwp[:, h, PKA:PKA + 1], scalar1=-1.0, scalar2=1.0,
                                                        op0=ALU.mult, op1=ALU.add), reads=[r_rc], writes=[r_rc])

            def P_(h, j):
                return rwp[:, h, j:j + 1]

            Hf = [kb.sb(es, [64, 64], F32, "Hf") for h in range(HR)]
            Hb = [kb.sb(es, [64, 64], BF16, "Hb") for h in range(HR)]
            r_H = [Reg() for h in range(HR)]
            for h in range(HR):
                kb.op("pool", lambda e, h=h: e.memset(Hf[h][:], 0.0), writes=[r_H[h]])
                kb.op("pool", lambda e, h=h: e.memset(Hb[h][:], 0.0), writes=[r_H[h]])
            raw = [[kb.sb(es, [64, 513], F32, "raw") for j in range(3)] for h in range(HR)]
            r_raw = [Reg() for h in range(HR)]
            rawl = [kb.sb(es, [64, 513], F32, "rawl") for j in range(2)]
            rawg = kb.sb(es, [128, 513], F32, "rawg")
            r_rawl = Reg()

            def ft(name, dt=F32, p=64):
                return kb.sb(es, [p, 512], dt, name)

            th, xab, sgx = ft("th", BF16), ft("xab", BF16), ft("sgx", BF16, 128)
            r_lo = Reg()
            tmpa, tmpg = ft("tmpa"), ft("tmpg", F32, 128)
            r_tmpa, r_tmpg = Reg(), Reg()
            sh_l = [ft("shl0"), ft("shl1"), ft("shg", F32, 128)]
            r_shl = Reg()
            rs_, ks_, vs_ = ft("rs_"), ft("ks_"), ft("vs_")
            lwp, clp, aa_, kk, kkn, kp, bet = ft("lwp"), ft("clp"), ft("aa_"), ft("kk"), ft("kkn"), ft("kp"), ft("bet")
            e1, e2, e3, e4 = ft("e1"), ft("e2"), ft("e3"), ft("e4")
            t1_, t2_ = ft("t1_"), ft("t2_")
            r_pre = Reg()
            gT = [ft("gT") for h in range(HR)]
            rkc = [ft("rkc") for h in range(HR)]
            vsk = [ft("vsk") for h in range(HR)]
            AT = [ft("AT", BF16) for h in range(HR)]
            BT = [ft("BT", BF16) for h in range(HR)]
            KTt = [ft("KTt", BF16) for h in range(HR)]
            RT = [ft("RT", BF16) for h in range(HR)]
            BhT = [ft("BhT", BF16) for h in range(HR)]
            KhT = [ft("KhT", BF16) for h in range(HR)]
            VT = [ft("VT", BF16) for h in range(HR)]
            g1c = [kb.sb(es, [64, 4], F32, "g1c") for h in range(HR)]
            r_fm = [Reg() for h in range(HR)]
            Ysb = [ft("Ysb") for h in range(HR)]
            r_Y = [Reg() for h in range(HR)]
            TOK = kb.sb(es, [128, HR, 4, 64], BF16, "TOK")
            r_TOK = Reg()
            L4 = [kb.sb(es, [128, 512], BF16, "L4") for h in range(HR)]
            r_L4 = [Reg() for h in range(HR)]
            X0 = kb.sb(es, [128, HR, 128], BF16, "X0")
            r_X0 = Reg()
            XX = [kb.sb(es, [128, HR, 256], BF16, "XX") for _ in range(2)]
            r_XX = [Reg(), Reg()]
            Z = [kb.sb(es, [128, HR, 128], BF16, "Z") for _ in range(2)]
            r_Z = [Reg(), Reg()]
            MTs = kb.sb(es, [64, HR, 64], F32, "MTs")
            Ns = kb.sb(es, [64, HR, 64], F32, "Ns")
            PTs = kb.sb(es, [64, HR, 128], BF16, "PTs")
            Yls = kb.sb(es, [64, HR, 128], F32, "Yls")
            r_prod = Reg()
            orw = [ft("orw", BF16) for _ in range(2)]
            r_orw = [Reg(), Reg()]
            pG = [kb.ps(es, [128, 512], F32, "pG") for _ in range(2)]
            r_pG = [Reg(), Reg()]
            pXX = kb.ps(es, [128, 1024], F32, "pXX")
            r_pXX = Reg()
            pZ = kb.ps(es, [128, 512], F32, "pZ")
            r_pZ = Reg()
            pM = kb.ps(es, [128, 512], F32, "pM")
            r_pM = Reg()
            pT = kb.ps(es, [128, 1024], BF16, "pT")
            r_pT = Reg()
            pS = kb.ps(es, [128, 512], F32, "pS")
            r_pS = Reg()
            r_mixr = Reg()
            NT = S // 512

            def shift(dst, src, mu, tmpt, r_tmpt, rd, wr):
                kb.op("pool", lambda e: e.tensor_sub(out=tmpt[:], in0=src[:, 0:512], in1=src[:, 1:513]), reads=[rd], writes=[r_tmpt])
                kb.op("dve", lambda e: e.scalar_tensor_tensor(out=dst[:], in0=tmpt[:], scalar=mu, in1=src[:, 1:513], op0=ALU.mult, op1=ALU.add),
                      reads=[r_tmpt, rd, r_rc], writes=[wr])

            def ldr(dst, row0, nrow, reg, t):
                c0 = t * 512
                if t == 0:
                    kb.op("pool", lambda e: e.memset(dst[:, 0:1], 0.0), writes=[reg])
                    kb.dma("sp", dst[:, 1:513], rwT_d[row0:row0 + nrow, 0:512], writes=[reg])
                else:
                    kb.dma("sp", dst[:, :], rwT_d[row0:row0 + nrow, c0 - 1:c0 + 512], writes=[reg])

            for t in range(NT):
                c0 = t * 512
                ldr(rawl[0], LW0, 64, r_rawl, t)
                ldr(rawl[1], LW0 + 64, 64, r_rawl, t)
                ldr(rawg, LG0, 128, r_rawl, t)
                shift(sh_l[0], rawl[0], lmu[0:64, 0:1], tmpa, r_tmpa, r_rawl, r_shl)
                shift(sh_l[1], rawl[1], lmu[0:64, 1:2], tmpa, r_tmpa, r_rawl, r_shl)
                shift(sh_l[2], rawg, lmu[:, 2:3], tmpg, r_tmpg, r_rawl, r_shl)
                kb.op("act", lambda e: e.activation(out=th[:], in_=sh_l[0][:], func=AF.Tanh), reads=[r_shl], writes=[r_lo])
                kb.op("act", lambda e: e.activation(out=xab[:], in_=sh_l[1][:], func=AF.Copy), reads=[r_shl], writes=[r_lo])
                kb.op("act", lambda e: e.activation(out=sgx[:], in_=sh_l[2][:], func=AF.Sigmoid), reads=[r_shl], writes=[r_lo])
                for h in range(HR):
                    hs = slice(h * 64, (h + 1) * 64)
                    ldr(raw[h][0], R0 + h * 64, 64, r_raw[h], t)
                    ldr(raw[h][1], K0 + h * 64, 64, r_raw[h], t)
                    ldr(raw[h][2], V0 + h * 64, 64, r_raw[h], t)
                    shift(rs_, raw[h][0], P_(h, PMU_R), tmpa, r_tmpa, r_raw[h], r_pre)
                    shift(ks_, raw[h][1], P_(h, PMU_K), tmpa, r_tmpa, r_raw[h], r_pre)
                    shift(vs_, raw[h][2], P_(h, PMU_V), tmpa, r_tmpa, r_raw[h], r_pre)
                    RD = [r_pre, r_rc]
                    kb.op("pe", lambda e: e.matmul(pS[0:64, :], lhsT=w2b[:, hs], rhs=th[:], start=True, stop=True), reads=[r_lo, r_rc], writes=[r_pS])
                    kb.op("act", lambda e: e.activation(out=lwp[:], in_=pS[0:64, :], func=AF.Sigmoid, bias=P_(h, PW0), scale=1.0), reads=[r_pS, r_rc], writes=[r_pre])
                    kb.op("pe", lambda e: e.matmul(pS[0:64, :], lhsT=a2b[:, hs], rhs=xab[:], start=True, stop=True), reads=[r_lo, r_rc], writes=[r_pS])
                    kb.op("act", lambda e: e.activation(out=aa_[:], in_=pS[0:64, :], func=AF.Sigmoid, bias=P_(h, PA0), scale=1.0), reads=[r_pS, r_rc], writes=[r_pre])
                    kb.op("pe", lambda e: e.matmul(pS[0:64, :], lhsT=g2b[:, hs], rhs=sgx[:], start=True, stop=True), reads=[r_lo, r_rc], writes=[r_pS])
                    kb.op("act", lambda e: e.activation(out=gT[h][:], in_=pS[0:64, :], func=AF.Copy), reads=[r_pS], writes=[r_fm[h]])
                    kb.op("dve", lambda e: e.tensor_tensor_scan(out=clp[:], data0=rmask[:], data1=lwp[:], initial=0.0, op0=ALU.mult, op1=ALU.add),
                          reads=RD, writes=[r_pre])
                    kb.op("act", lambda e: e.activation(out=e1[:], in_=clp[:], func=AF.Exp, scale=-E05), reads=RD, writes=[r_pre])
                    kb.op("act", lambda e: e.activation(out=e2[:], in_=clp[:], func=AF.Exp, scale=E05), reads=RD, writes=[r_pre])
                    kb.op("pool", lambda e: e.tensor_sub(out=t1_[:], in0=clp[:], in1=lwp[:]), reads=RD, writes=[r_pre])
                    kb.op("act", lambda e: e.activation(out=e3[:], in_=t1_[:], func=AF.Exp, scale=-E05), reads=RD, writes=[r_pre])
                    clp3 = clp[:].rearrange("p (c j) -> p c j", j=128)
                    kb.op("pool", lambda e: e.tensor_sub(out=t2_[:].rearrange("p (c j) -> p c j", j=128), in0=clp3[:, :, 127:128].broadcast_to([64, 4, 128]),
                                                        in1=clp3), reads=RD, writes=[r_pre])
                    kb.op("act", lambda e: e.activation(out=e4[:], in_=t2_[:], func=AF.Exp, scale=-E05), reads=RD, writes=[r_pre])
                    kb.op("dve", lambda e: e.tensor_copy(out=g1c[h][:], in_=e1[:].rearrange("p (c j) -> p c j", j=128)[:, :, 127]), reads=RD, writes=[r_fm[h]])
                    kb.op("dve", lambda e: e.tensor_scalar(out=kk[:], in0=ks_[:], scalar1=P_(h, PKK), scalar2=None, op0=ALU.mult), reads=RD, writes=[r_pre])
                    kb.op("act", lambda e: e.activation(out=t1_[:], in_=kk[:], func=AF.Square), reads=RD, writes=[r_pre])
                    kb.op("pe", lambda e: e.matmul(pS[0:64, :], lhsT=ones64[:], rhs=t1_[:], start=True, stop=True), reads=RD, writes=[r_pS])
                    kb.op("act", lambda e: e.activation(out=t2_[:], in_=pS[0:64, :], func=AF.Sqrt), reads=[r_pS], writes=[r_pre])
                    kb.op("dve", lambda e: e.tensor_scalar(out=t2_[:], in0=t2_[:], scalar1=1e-12, scalar2=None, op0=ALU.max), reads=RD, writes=[r_pre])
                    kb.op("dve", lambda e: e.reciprocal(out=t2_[:], in_=t2_[:]), reads=RD, writes=[r_pre])
                    kb.op("dve", lambda e: e.tensor_tensor(out=kkn[:], in0=kk[:], in1=t2_[:], op=ALU.mult), reads=RD, writes=[r_pre])
                    kb.op("dve", lambda e: e.tensor_scalar(out=t1_[:], in0=aa_[:], scalar1=P_(h, PKA), scalar2=P_(h, POMKA), op0=ALU.mult, op1=ALU.add),
                          reads=RD, writes=[r_pre])
                    kb.op("pool", lambda e: e.tensor_tensor(out=kp[:], in0=ks_[:], in1=t1_[:], op=ALU.mult), reads=RD, writes=[r_pre])
                    kb.op("pool", lambda e: e.tensor_tensor(out=bet[:], in0=kkn[:], in1=aa_[:], op=ALU.mult), reads=RD, writes=[r_pre])
                    W_ = [r_fm[h]]
                    kb.op("dve", lambda e: e.scalar_tensor_tensor(out=AT[h][:], in0=kkn[:], scalar=-1.0, in1=e3[:], op0=ALU.mult, op1=ALU.mult), reads=RD, writes=W_)
                    kb.op("pool", lambda e: e.tensor_tensor(out=BT[h][:], in0=bet[:], in1=e2[:], op=ALU.mult), reads=RD, writes=W_)
                    kb.op("pool", lambda e: e.tensor_tensor(out=BhT[h][:], in0=bet[:], in1=e4[:], op=ALU.mult), reads=RD, writes=W_)
                    kb.op("dve", lambda e: e.tensor_tensor(out=KTt[h][:], in0=kp[:], in1=e2[:], op=ALU.mult), reads=RD, writes=W_)
                    kb.op("pool", lambda e: e.tensor_tensor(out=KhT[h][:], in0=kp[:], in1=e4[:], op=ALU.mult), reads=RD, writes=W_)
                    kb.op("dve", lambda e: e.tensor_tensor(out=RT[h][:], in0=rs_[:], in1=e1[:], op=ALU.mult), reads=RD, writes=W_)
                    kb.op("act", lambda e: e.activation(out=VT[h][:], in_=vs_[:], func=AF.Copy), reads=RD, writes=W_)
                    kb.op("pool", lambda e: e.tensor_copy(out=vsk[h][:], in_=vs_[:]), reads=RD, writes=W_)
                    kb.op("dve", lambda e: e.scalar_tensor_tensor(out=t1_[:], in0=rs_[:], scalar=P_(h, PRK), in1=kp[:], op0=ALU.mult, op1=ALU.mult), reads=RD, writes=[r_pre])
                    kb.op("pe", lambda e: e.matmul(pS[0:64, :], lhsT=ones64[:], rhs=t1_[:], start=True, stop=True), reads=RD, writes=[r_pS])
                    kb.op("act", lambda e: e.activation(out=rkc[h][:], in_=pS[0:64, :], func=AF.Copy), reads=[r_pS], writes=W_)
                for ci in range(4):
                    cs = slice(ci * 128, (ci + 1) * 128)
                    for h in range(HR):
                        for j, src in enumerate((AT, BhT, KhT, VT)):
                            kb.op("pe", lambda e, h=h, j=j, src=src: e.transpose(out=pT[:, (h * 4 + j) * 64:(h * 4 + j + 1) * 64], in_=src[h][:, cs], identity=ident[0:64, 0:64]),
                                  reads=[r_fm[h], r_const], writes=[r_pT])
                    kb.op("act", lambda e: e.activation(out=TOK[:].rearrange("p h j c -> p (h j c)"), in_=pT[:, 0:HR * 256], func=AF.Copy), reads=[r_pT], writes=[r_TOK])
                    for h in range(HR):
                        gb = h % 2
                        for j, (l_, r__) in enumerate(((BT, AT), (BT, RT), (KTt, AT), (KTt, RT))):
                            kb.op("pe", lambda e, h=h, j=j, l_=l_, r__=r__: e.matmul(pG[gb][:, j * 128:(j + 1) * 128], lhsT=l_[h][:, cs], rhs=r__[h][:, cs],
                                                                                  start=(j == 0), stop=True, skip_group_check=True), reads=[r_fm[h]], writes=[r_pG[gb]])
                        kb.op("dve", lambda e, h=h, gb=gb: e.tensor_tensor(out=L4[h][:], in0=pG[gb][:], in1=mask4[:], op=ALU.mult), reads=[r_pG[gb], r_rc], writes=[r_L4[h]])
                    for h in range(HR):
                        kb.op("pe", lambda e, h=h: e.matmul(pM[:, h * 128:(h + 1) * 128], lhsT=AT[h][:, cs], rhs=BT[h][:, cs], start=(h == 0), stop=True,
                                                          skip_group_check=True), reads=[r_fm[h]], writes=[r_pM])
                    kb.op("dve", lambda e: e.tensor_tensor(out=X0[:], in0=pM[:, 0:HR * 128].rearrange("p (h s) -> p h s", h=HR),
                                                          in1=masksl[:].unsqueeze(1).broadcast_to([128, HR, 128]), op=ALU.mult), reads=[r_pM, r_rc], writes=[r_X0])
                    for h in range(HR):
                        kb.op("pe", lambda e, h=h: e.matmul(pZ[:, h * 64:(h + 1) * 64], lhsT=L4[h][:, 256:384], rhs=TOK[:, h, 3, :], start=(h == 0), stop=True,
                                                          skip_group_check=True), reads=[r_L4[h], r_TOK], writes=[r_pZ])
                    kb.op("act", lambda e: e.activation(out=Z[0][:, :, 64:128], in_=pZ[:, 0:HR * 64].rearrange("p (h c) -> p h c", h=HR), func=AF.Copy),
                          reads=[r_pZ], writes=[r_Z[0]])
                    kb.op("pool", lambda e: e.tensor_copy(out=Z[0][:, :, 0:64], in_=TOK[:, :, 0, :]), reads=[r_TOK], writes=[r_Z[0]])
                    zc = 0
                    for lev in range(7):
                        def Xk(h):
                            return X0[:, h, :] if lev == 0 else XX[(lev - 1) % 2][:, h, 0:128]

                        def XTk(h):
                            return L4[h][:, 0:128] if lev == 0 else XX[(lev - 1) % 2][:, h, 128:256]
                        rx = [r_X0] + r_L4 if lev == 0 else [r_XX[(lev - 1) % 2]]
                        for h in range(HR):
                            kb.op("pe", lambda e, h=h: e.matmul(pZ[:, h * 128:(h + 1) * 128], lhsT=XTk(h), rhs=Z[zc][:, h, :], start=(h == 0), stop=True,
                                                              skip_group_check=True), reads=rx + [r_Z[zc]], writes=[r_pZ])
                        if lev < 6:
                            for h in range(HR):
                                kb.op("pe", lambda e, h=h: e.matmul(pXX[:, h * 256:h * 256 + 128], lhsT=XTk(h), rhs=Xk(h), start=(h % 2 == 0), stop=True,
                                                                  skip_group_check=True), reads=rx, writes=[r_pXX])
                                kb.op("pe", lambda e, h=h: e.matmul(pXX[:, h * 256 + 128:h * 256 + 256], lhsT=Xk(h), rhs=XTk(h), start=False, stop=True,
                                                                  skip_group_check=True), reads=rx, writes=[r_pXX])
                        kb.op("dve", lambda e: e.tensor_tensor(out=Z[1 - zc][:], in0=pZ[:, 0:HR * 128].rearrange("p (h c) -> p h c", h=HR), in1=Z[zc][:], op=ALU.add),
                              reads=[r_pZ, r_Z[zc]], writes=[r_Z[1 - zc]])
                        zc = 1 - zc
                        if lev < 6:
                            kb.op("act", lambda e: e.activation(out=XX[lev % 2][:].rearrange("p h c -> p (h c)"), in_=pXX[:, 0:HR * 256], func=AF.Copy),
                                  reads=[r_pXX], writes=[r_XX[lev % 2]])
                    Wt = Z[zc]
                    rW = r_Z[zc]
                    for h in range(HR):
                        kb.op("pe", lambda e, h=h: e.matmul(pM[0:64, h * 64:(h + 1) * 64], lhsT=Wt[:, h, 0:64], rhs=TOK[:, h, 1, :], start=(h == 0), stop=True,
                                                          skip_group_check=True), reads=[rW, r_TOK], writes=[r_pM])
                    for h in range(HR):
                        o_ = HR * 64 + h * 64
                        kb.op("pe", lambda e, h=h, o_=o_: e.matmul(pM[0:64, o_:o_ + 64], lhsT=TOK[:, h, 1, :], rhs=Wt[:, h, 64:128], start=False, stop=False,
                                                                 skip_group_check=True), reads=[rW, r_TOK], writes=[r_pM])
                        kb.op("pe", lambda e, h=h, o_=o_: e.matmul(pM[0:64, o_:o_ + 64], lhsT=TOK[:, h, 2, :], rhs=TOK[:, h, 3, :], start=False, stop=True,
                                                                 skip_group_check=True), reads=[rW, r_TOK], writes=[r_pM])
                    for h in range(HR):
                        kb.op("pe", lambda e, h=h: e.matmul(pXX[0:64, h * 128:(h + 1) * 128], lhsT=Wt[:, h, 0:64], rhs=L4[h][:, 128:256], start=(h == 0), stop=True,
                                                          skip_group_check=True), reads=[rW, r_L4[h]], writes=[r_pXX])
                    for h in range(HR):
                        o_ = 512 + h * 128
                        kb.op("pe", lambda e, h=h, o_=o_: e.matmul(pXX[0:64, o_:o_ + 128], lhsT=Wt[:, h, 64:128], rhs=L4[h][:, 128:256], start=(h == 0), stop=False,
                                                                 skip_group_check=True), reads=[rW, r_L4[h]], writes=[r_pXX])
                        kb.op("pe", lambda e, h=h, o_=o_: e.matmul(pXX[0:64, o_:o_ + 128], lhsT=TOK[:, h, 3, :], rhs=L4[h][:, 384:512], start=False, stop=True,
                                                                 skip_group_check=True), reads=[r_TOK, r_L4[h]], writes=[r_pXX])
                    for h in range(HR):
                        kb.op("dve", lambda e, h=h: e.scalar_tensor_tensor(out=MTs[:, h, :], in0=idf[:], scalar=g1c[h][:, ci:ci + 1], in1=pM[0:64, h * 64:(h + 1) * 64],
                                                                         op0=ALU.mult, op1=ALU.add), reads=[r_pM, r_rc, r_fm[h]], writes=[r_prod])
                        kb.op("dve", lambda e, h=h: e.tensor_tensor(out=PTs[:, h, :], in0=pXX[0:64, h * 128:(h + 1) * 128], in1=RT[h][:, cs], op=ALU.add),
                              reads=[r_pXX, r_fm[h]], writes=[r_prod])
                    kb.op("act", lambda e: e.activation(out=Ns[:].rearrange("p h c -> p (h c)"), in_=pM[0:64, HR * 64:2 * HR * 64], func=AF.Copy), reads=[r_pM], writes=[r_prod])
                    kb.op("act", lambda e: e.activation(out=Yls[:].rearrange("p h c -> p (h c)"), in_=pXX[0:64, 512:512 + HR * 128], func=AF.Copy), reads=[r_pXX], writes=[r_prod])
                    for h in range(HR):
                        kb.op("pe", lambda e, h=h: e.matmul(pG[0][0:64, h * 128:(h + 1) * 128], lhsT=Hb[h][:], rhs=PTs[:, h, :], start=(h == 0), stop=True,
                                                          skip_group_check=True), reads=[r_H[h], r_prod], writes=[r_pG[0]])
                    for h in range(HR):
                        kb.op("dve", lambda e, h=h: e.tensor_tensor(out=Ysb[h][:, cs], in0=pG[0][0:64, h * 128:(h + 1) * 128], in1=Yls[:, h, :], op=ALU.add),
                              reads=[r_pG[0], r_prod], writes=[r_Y[h]])
                    for h in range(HR):
                        kb.op("pe", lambda e, h=h: e.matmul(pG[1][0:64, h * 64:(h + 1) * 64], lhsT=MTs[:, h, :], rhs=Hf[h][:], start=(h == 0), stop=True,
                                                          skip_group_check=True), reads=[r_H[h], r_prod], writes=[r_pG[1]])
                    for h in range(HR):
                        kb.op("dve", lambda e, h=h: e.tensor_tensor(out=Hf[h][:], in0=pG[1][0:64, h * 64:(h + 1) * 64], in1=Ns[:, h, :], op=ALU.add),
                              reads=[r_pG[1], r_prod], writes=[r_H[h]])
                        kb.op("act", lambda e, h=h: e.activation(out=Hb[h][:], in_=Hf[h][:], func=AF.Copy), reads=[r_H[h]], writes=[r_H[h]])
                for h in range(HR):
                    RY = [r_Y[h], r_fm[h], r_rc]
                    kb.op("pe", lambda e, h=h: e.matmul(pS[0:64, :], lhsT=ones64[:], rhs=Ysb[h][:], start=True, stop=True), reads=RY, writes=[r_pS])
                    kb.op("dve", lambda e, h=h: e.scalar_tensor_tensor(out=t1_[:], in0=pS[0:64, :], scalar=-1.0 / 64, in1=Ysb[h][:], op0=ALU.mult, op1=ALU.add),
                          reads=[r_pS] + RY, writes=[r_pre])
                    kb.op("act", lambda e: e.activation(out=t2_[:], in_=t1_[:], func=AF.Square), reads=[r_pre], writes=[r_pre])
                    kb.op("pe", lambda e: e.matmul(pS[0:64, :], lhsT=ones64[:], rhs=t2_[:], start=True, stop=True), reads=[r_pre, r_rc], writes=[r_pS])
                    kb.op("act", lambda e: e.activation(out=t2_[:], in_=pS[0:64, :], func=AF.Sqrt, bias=GN_EPS, scale=1.0 / 64), reads=[r_pS], writes=[r_pre])
                    kb.op("dve", lambda e: e.reciprocal(out=t2_[:], in_=t2_[:]), reads=[r_pre], writes=[r_pre])
                    kb.op("dve", lambda e: e.tensor_tensor(out=t1_[:], in0=t1_[:], in1=t2_[:], op=ALU.mult), reads=[r_pre], writes=[r_pre])
                    kb.op("dve", lambda e, h=h: e.tensor_scalar(out=t1_[:], in0=t1_[:], scalar1=P_(h, PGW), scalar2=P_(h, PGB), op0=ALU.mult, op1=ALU.add),
                          reads=[r_pre, r_rc], writes=[r_pre])
                    kb.op("pool", lambda e, h=h: e.tensor_tensor(out=t2_[:], in0=rkc[h][:], in1=vsk[h][:], op=ALU.mult), reads=RY + [r_pre], writes=[r_pre])
                    kb.op("pool", lambda e: e.tensor_tensor(out=t1_[:], in0=t1_[:], in1=t2_[:], op=ALU.add), reads=[r_pre], writes=[r_pre])
                    ob = (t * HR + h) % 2
                    kb.op("dve", lambda e, h=h, ob=ob: e.tensor_tensor(out=orw[ob][:], in0=t1_[:], in1=gT[h][:], op=ALU.mult), reads=[r_pre] + RY, writes=[r_orw[ob]])
                    kb.dma("sp", mixT_d[cfg.OFF_RW + h * 64:cfg.OFF_RW + (h + 1) * 64, c0:c0 + 512], orw[ob][:], reads=[r_orw[ob]], writes=[r_mixr])
            kb.barrier()

        if cfg.rw_zero:
          with ExitStack() as es:
            zt = kb.sb(es, [128, 2048], BF16, "zt")
            r_zt = Reg()
            kb.op("pool", lambda e: e.memset(zt[:], 0.0), writes=[r_zt])
            r_zo = Reg()
            for c in range(HR * 64 // 128):
                for t in range(S // 2048):
                    kb.dma("sp", mixT_d[cfg.OFF_RW + c * 128:cfg.OFF_RW + (c + 1) * 128, t * 2048:(t + 1) * 2048], zt[:], reads=[r_zt], writes=[r_zo])
            kb.barrier()

        if cfg.do_tail:
          with ExitStack() as es:
            TBK = 1024
            NSUB = TBK // 128
            NBLK = cfg.TT // TBK
            wout = kb.sb(es, [128, 8, D], BF16, "wout")
            wos = [kb.sb(es, [128, D], F32, "wos") for _ in range(2)]
            r_wos = [Reg(), Reg()]
            r_wout = Reg()
            wosc = kb.sb(es, [128, 8], F32, "wosc")
            fg = kb.sb(es, [128, 8], F32, "fg")
            rbias = kb.sb(es, [128, 20], F32, "rbias")
            wr = kb.sb(es, [128, 8, 20], BF16, "wr")
            r_tp = Reg()
            kb.dma("sp", wosc[:], wosc_d, writes=[r_tp])
            kb.dma("sp", fg[:], fnorm_d, writes=[r_tp])
            kb.dma("sp", rbias[:], rbias_d.partition_broadcast(128), writes=[r_tp])
            kb.dma("pool", wr[:], wr_d.rearrange("(kc p) n -> p kc n", p=128), writes=[r_tp])
            for kc in range(8):
                b = kc % 2
                kb.dma("sp", wos[b][:], wout_d[kc * 128:(kc + 1) * 128, :], writes=[r_wos[b]])
                kb.op("dve", lambda e, kc=kc, b=b: e.tensor_scalar(out=wout[:, kc, :], in0=wos[b][:], scalar1=wosc[:, kc:kc + 1], scalar2=None,
                                                              op0=ALU.mult), reads=[r_wos[b], r_tp], writes=[r_wout])
            yacc = kb.sb(es, [128, NSUB, D], F32, "yacc")
            r_y = [Reg() for _ in range(NSUB)]
            xnT = kb.sb(es, [128, 8, TBK], BF16, "xnT")
            r_xnT = Reg()
            gate = kb.sb(es, [128, NSUB, NE], F32, "gate")
            r_gate = [Reg() for _ in range(NSUB)]
            mixt = [kb.sb(es, [128, 8, 512], BF16, "mixt") for _ in range(2)]
            r_mixt = [Reg(), Reg()]
            xtt = [kb.sb(es, [128, 4, D], F32, "xtt") for _ in range(2)]
            r_xtt = [Reg(), Reg()]
            xsb = kb.sb(es, [128, D], BF16, "xsb")
            r_xsb = Reg()
            tjunk = kb.sb(es, [128, D], BF16, "tjunk")
            r_tj = Reg()
            sm = kb.sb(es, [128, 64], F32, "sm")
            r_sm = Reg()
            wg = [kb.sb(es, [128, 8, DE], BF16, "wg") for _ in range(2)]
            wu = [kb.sb(es, [128, 8, DE], BF16, "wu") for _ in range(2)]
            wd = [kb.sb(es, [128, 4, D], BF16, "wd") for _ in range(2)]
            r_we = [Reg(), Reg()]
            hd = [kb.sb(es, [128, 4, 512], BF16, "hd") for _ in range(2)]
            r_hd = [Reg(), Reg()]
            sg = [kb.sb(es, [128, 512], F32, "sg") for _ in range(2)]
            r_sg = [Reg(), Reg()]
            py = kb.ps(es, [128, D], F32, "py")
            r_py = [Reg(), Reg()]
            pg = [kb.ps(es, [128, 512], F32, "pg") for _ in range(2)]
            r_pg = [Reg(), Reg()]
            pu = [kb.ps(es, [128, 512], F32, "pu") for _ in range(2)]
            r_pu = [Reg(), Reg()]
            ptt = kb.ps(es, [128, 8, 128], BF16, "ptt")
            r_ptt = Reg()
            plg = kb.ps(es, [128, 512], F32, "plg")
            r_plg = Reg()
            r_o = Reg()
            xo_v = xo_d.rearrange("(t s p) d -> t p s d", s=4, p=128)
            out_v = out_d.rearrange("(k s p) d -> k p s d", s=NSUB, p=128)
            nw = 0

            def load_expert(e_, b):
                kb.dma("pool", wg[b][:], wge_d[e_].rearrange("(kc p) f -> p kc f", p=128), writes=[r_we[b]])
                kb.dma("pool", wu[b][:], wue_d[e_].rearrange("(kc p) f -> p kc f", p=128), writes=[r_we[b]])
                kb.dma("pool", wd[b][:], wde_d[e_].rearrange("(kc p) f -> p kc f", p=128), writes=[r_we[b]])

            for blk_i in range(NBLK):
                for tt in range(TBK // 512):
                    tg = blk_i * (TBK // 512) + tt
                    mb = tg % 2
                    c0 = cfg.tail_tok0 + tg * 512
                    kb.dma("sp", mixt[mb][:], mixT_d[:, c0:c0 + 512].rearrange("(kc p) t -> p kc t", p=128), writes=[r_mixt[mb]])
                    kb.dma("sp", xtt[mb][:], xo_v[tg], writes=[r_xtt[mb]])
                    for s_ in range(4):
                        sub = tt * 4 + s_
                        for half in range(2):
                            for kc in range(8):
                                kb.op("pe", lambda e, s_=s_, half=half, kc=kc: e.matmul(
                                    py[:, half * 512:(half + 1) * 512], lhsT=mixt[mb][:, kc, s_ * 128:(s_ + 1) * 128],
                                    rhs=wout[:, kc, half * 512:(half + 1) * 512], start=(kc == 0), stop=(kc == 7)),
                                    reads=[r_mixt[mb], r_wout], writes=[r_py[half]])
                            kb.op("dve", lambda e, s_=s_, half=half, sub=sub: e.tensor_tensor(
                                out=yacc[:, sub, half * 512:(half + 1) * 512], in0=py[:, half * 512:(half + 1) * 512],
                                in1=xtt[mb][:, s_, half * 512:(half + 1) * 512], op=ALU.add),
                                reads=[r_py[half], r_xtt[mb]], writes=[r_y[sub]])
                        kb.op("act", lambda e, sub=sub: e.activation(out=tjunk[:], in_=yacc[:, sub, :], func=AF.Square, accum_out=sm[:, 0:1]),
                              reads=[r_y[sub]], writes=[r_tj, r_sm])
                        kb.op("act", lambda e: e.activation(out=sm[:, 1:2], in_=sm[:, 0:1], func=AF.Sqrt, bias=EPS, scale=1.0 / D),
                              reads=[r_sm], writes=[r_sm])
                        kb.op("dve", lambda e: e.reciprocal(out=sm[:, 2:3], in_=sm[:, 1:2]), reads=[r_sm], writes=[r_sm])
                        kb.op("dve", lambda e, sub=sub: e.tensor_scalar(out=xsb[:], in0=yacc[:, sub, :], scalar1=sm[:, 2:3], scalar2=None, op0=ALU.mult),
                              reads=[r_y[sub], r_sm], writes=[r_xsb])
                        for kc in range(8):
                            kb.op("pe", lambda e, kc=kc: e.transpose(out=ptt[:, kc, :], in_=xsb[:, kc * 128:(kc + 1) * 128], identity=ident[:]),
                                  reads=[r_xsb, r_const], writes=[r_ptt])
                        kb.op("dve", lambda e, sub=sub: e.tensor_tensor(out=xnT[:, :, sub * 128:(sub + 1) * 128], in0=ptt[:],
                                                                      in1=fg[:].unsqueeze(2).broadcast_to([128, 8, 128]), op=ALU.mult),
                              reads=[r_ptt, r_tp], writes=[r_xnT])
                        for kc in range(8):
                            kb.op("pe", lambda e, kc=kc, sub=sub: e.matmul(plg[:, 0:20], lhsT=xnT[:, kc, sub * 128:(sub + 1) * 128], rhs=wr[:, kc, :],
                                                                        start=(kc == 0), stop=(kc == 7)), reads=[r_xnT, r_tp], writes=[r_plg])
                        L = sm[:, 8:28]
                        G_, E_ = sm[:, 8:12], sm[:, 12:28]
                        kb.op("dve", lambda e: e.tensor_tensor(out=L, in0=plg[:, 0:20], in1=rbias[:], op=ALU.add), reads=[r_plg, r_tp], writes=[r_sm])
                        kb.op("dve", lambda e: e.tensor_reduce(out=sm[:, 3:4], in_=G_, axis=AX.X, op=ALU.max), reads=[r_sm], writes=[r_sm])
                        kb.op("dve", lambda e: e.tensor_scalar(out=sm[:, 4:5], in0=sm[:, 3:4], scalar1=-1.0, scalar2=None, op0=ALU.mult), reads=[r_sm], writes=[r_sm])
                        kb.op("act", lambda e: e.activation(out=sm[:, 28:32], in_=G_, func=AF.Exp, bias=sm[:, 4:5], scale=1.0, accum_out=sm[:, 5:6]),
                              reads=[r_sm], writes=[r_sm])
                        kb.op("dve", lambda e: e.reciprocal(out=sm[:, 6:7], in_=sm[:, 5:6]), reads=[r_sm], writes=[r_sm])
                        kb.op("dve", lambda e: e.tensor_scalar(out=sm[:, 28:32], in0=G_, scalar1=sm[:, 3:4], scalar2=None, op0=ALU.is_equal), reads=[r_sm], writes=[r_sm])
                        kb.op("dve", lambda e: e.tensor_scalar(out=sm[:, 28:32], in0=sm[:, 28:32], scalar1=-1.0, scalar2=1e30, op0=ALU.add, op1=ALU.mult),
                              reads=[r_sm], writes=[r_sm])
                        EM = sm[:, 32:48]
                        kb.op("dve", lambda e: e.tensor_tensor(out=EM.rearrange("p (g k) -> p g k", g=4), in0=E_.rearrange("p (g k) -> p g k", g=4),
                                                              in1=sm[:, 28:32].unsqueeze(2).broadcast_to([128, 4, 4]), op=ALU.add), reads=[r_sm], writes=[r_sm])
                        kb.op("dve", lambda e: e.tensor_reduce(out=sm[:, 7:8], in_=EM, axis=AX.X, op=ALU.max), reads=[r_sm], writes=[r_sm])
                        OH1 = sm[:, 48:64]
                        kb.op("dve", lambda e: e.tensor_scalar(out=OH1, in0=EM, scalar1=sm[:, 7:8], scalar2=None, op0=ALU.is_equal), reads=[r_sm], writes=[r_sm])
                        kb.op("dve", lambda e: e.scalar_tensor_tensor(out=EM, in0=OH1, scalar=-1e30, in1=EM, op0=ALU.mult, op1=ALU.add), reads=[r_sm], writes=[r_sm])
                        kb.op("dve", lambda e: e.tensor_reduce(out=sm[:, 0:1], in_=EM, axis=AX.X, op=ALU.max), reads=[r_sm], writes=[r_sm])
                        OH2 = sm[:, 8:24]
                        kb.op("dve", lambda e: e.tensor_scalar(out=OH2, in0=EM, scalar1=sm[:, 0:1], scalar2=None, op0=ALU.is_equal), reads=[r_sm], writes=[r_sm])
                        kb.op("dve", lambda e: e.tensor_sub(out=sm[:, 1:2], in0=sm[:, 0:1], in1=sm[:, 7:8]), reads=[r_sm], writes=[r_sm])
                        kb.op("act", lambda e: e.activation(out=sm[:, 2:3], in_=sm[:, 1:2], func=AF.Exp), reads=[r_sm], writes=[r_sm])
                        kb.op("dve", lambda e: e.tensor_scalar(out=sm[:, 3:4], in0=sm[:, 2:3], scalar1=1.0, scalar2=None, op0=ALU.add), reads=[r_sm], writes=[r_sm])
                        kb.op("dve", lambda e: e.reciprocal(out=sm[:, 3:4], in_=sm[:, 3:4]), reads=[r_sm], writes=[r_sm])
                        kb.op("dve", lambda e: e.tensor_mul(out=sm[:, 3:4], in0=sm[:, 3:4], in1=sm[:, 6:7]), reads=[r_sm], writes=[r_sm])
                        kb.op("dve", lambda e: e.tensor_mul(out=sm[:, 4:5], in0=sm[:, 3:4], in1=sm[:, 2:3]), reads=[r_sm], writes=[r_sm])
                        kb.op("dve", lambda e: e.tensor_scalar(out=OH1, in0=OH1, scalar1=sm[:, 3:4], scalar2=None, op0=ALU.mult), reads=[r_sm], writes=[r_sm])
                        kb.op("dve", lambda e, sub=sub: e.scalar_tensor_tensor(out=gate[:, sub, :], in0=OH2, scalar=sm[:, 4:5], in1=OH1, op0=ALU.mult, op1=ALU.add),
                              reads=[r_sm], writes=[r_gate[sub]])
                if blk_i == 0:
                    load_expert(0, 0)
                for e_ in range(NE):
                    wb = nw % 2
                    nw += 1
                    nxt = (blk_i * NE + e_ + 1)
                    if nxt < NBLK * NE:
                        load_expert(nxt % NE, nw % 2)
                    for tt in range(TBK // 512):
                        hb = (e_ * (TBK // 512) + tt) % 2
                        for fc in range(4):
                            gb = fc % 2
                            for kc in range(8):
                                kb.op("pe", lambda e, fc=fc, kc=kc, gb=gb: e.matmul(pg[gb][:], lhsT=wg[wb][:, kc, fc * 128:(fc + 1) * 128],
                                                                                 rhs=xnT[:, kc, tt * 512:(tt + 1) * 512], start=(kc == 0), stop=(kc == 7)),
                                      reads=[r_we[wb], r_xnT], writes=[r_pg[gb]])
                            for kc in range(8):
                                kb.op("pe", lambda e, fc=fc, kc=kc, gb=gb: e.matmul(pu[gb][:], lhsT=wu[wb][:, kc, fc * 128:(fc + 1) * 128],
                                                                                 rhs=xnT[:, kc, tt * 512:(tt + 1) * 512], start=(kc == 0), stop=(kc == 7)),
                                      reads=[r_we[wb], r_xnT], writes=[r_pu[gb]])
                            kb.op("act", lambda e, gb=gb: e.activation(out=sg[gb][:], in_=pg[gb][:], func=AF.Silu), reads=[r_pg[gb]], writes=[r_sg[gb]])
                            kb.op("dve", lambda e, fc=fc, gb=gb: e.tensor_tensor(out=hd[hb][:, fc, :], in0=pu[gb][:], in1=sg[gb][:], op=ALU.mult),
                                  reads=[r_pu[gb], r_sg[gb]], writes=[r_hd[hb]])
                        for s_ in range(4):
                            sub = tt * 4 + s_
                            for half in range(2):
                                for fc in range(4):
                                    kb.op("pe", lambda e, s_=s_, half=half, fc=fc: e.matmul(
                                        py[:, half * 512:(half + 1) * 512], lhsT=hd[hb][:, fc, s_ * 128:(s_ + 1) * 128],
                                        rhs=wd[wb][:, fc, half * 512:(half + 1) * 512], start=(fc == 0), stop=(fc == 3)),
                                        reads=[r_hd[hb], r_we[wb]], writes=[r_py[half]])
                                kb.op("dve", lambda e, half=half, sub=sub, e_=e_: e.scalar_tensor_tensor(
                                    out=yacc[:, sub, half * 512:(half + 1) * 512], in0=py[:, half * 512:(half + 1) * 512],
                                    scalar=gate[:, sub, e_:e_ + 1], in1=yacc[:, sub, half * 512:(half + 1) * 512], op0=ALU.mult, op1=ALU.add),
                                    reads=[r_py[half], r_gate[sub], r_y[sub]], writes=[r_y[sub]])
                kb.dma("sp", out_v[blk_i], yacc[:], reads=r_y, writes=[r_o])
            kb.barrier()
        kb.barrier()
    return nc


_SU = np.triu(np.ones((128, 128), np.float32), 1)
_SI = np.triu(np.ones((128, 128), np.float32), 0)
_MASK4 = np.ascontiguousarray(np.concatenate([_SU, _SI, _SU, _SI], axis=1))
_MASKSL = np.ascontiguousarray(np.tril(np.ones((128, 128), np.float32), -1))
_RMASK = np.ones((64, 512), np.float32)
_RMASK[:, ::128] = 0.0


def _rwp(inp, heads_rw):
    l = 0
    mu = inp["rw_mu"][l]
    cols = []
    for h in heads_rw:
        hs = slice(h * 64, (h + 1) * 64)
        cols.append(np.stack([mu[0:256][hs], mu[256:512][hs], mu[512:768][hs], inp["rw_w0"][l][hs], inp["rw_a0"][l][hs], inp["rw_k_k"][l][hs],
                              inp["rw_k_a"][l][hs], inp["rw_r_k"][l].reshape(-1)[hs], inp["rw_gn_w"][l][hs], inp["rw_gn_b"][l][hs]], axis=1))
    return np.ascontiguousarray(np.stack(cols, axis=1).astype(np.float32))


def _lmu(inp):
    mu = inp["rw_mu"][0]
    out = np.zeros((128, 3), np.float32)
    out[:64, 0] = mu[768:832]
    out[:64, 1] = mu[832:896]
    out[:, 2] = mu[896:1024]
    return out


def _core_inputs(cfg, inp, b, heads_da, heads_rw, heads_mem, tail_tok0=0):
    l = 0
    mixrows = ([h * 128 + e for h in heads_da for e in range(128)] + [512 + h * 64 + c for h in heads_rw for c in range(64)]
               + [768 + h * 64 + d for h in heads_mem for d in range(64)])
    if len(mixrows) < 1024:
        mixrows = mixrows + [r for r in range(1024) if r not in set(mixrows)]
    w_in = inp["w_in"][l]
    cols = []
    for h in heads_da:
        cols += list(range(h * 128, (h + 1) * 128))
        cols += list(range(512 + h * 128, 512 + (h + 1) * 128))
    for h in heads_mem:
        cols += list(range(2560 + h * 64, 2560 + (h + 1) * 64))
    rw0 = 1536
    for part in range(3):
        for h in heads_rw:
            cols += list(range(rw0 + part * 256 + h * 64, rw0 + part * 256 + (h + 1) * 64))
    cols += list(range(rw0 + 768, rw0 + 1024))
    for h in heads_da:
        cols += list(range(1024 + h * 128, 1024 + (h + 1) * 128))
    win = np.ascontiguousarray(w_in[:, cols])
    assert win.shape[1] == cfg.NCOL
    qg = inp["da_q_norm"][l].reshape(128)
    kg = inp["da_k_norm"][l].reshape(128)
    mg = np.concatenate([inp["mem_q_norm"][l], inp["mem_q_norm"][l]])
    qkg = np.stack([qg if (c % 2 == 0) else kg for c in range(cfg.NQK)] + [mg] * cfg.NMQ, axis=1).astype(np.float32)
    slopes = np.tile(np.array([[2.0 ** (-8.0 * (h + 1) / 4) for h in heads_da]], np.float32), (128, 1))
    blk = np.zeros((128, 128), np.float32)
    blk[:64, :64] = 1.0 / 64
    blk[64:, 64:] = 1.0 / 64
    return dict(
        x=np.ascontiguousarray(inp["x"][b]),
        pos=np.ascontiguousarray(inp["positions"][b:b + 1]).astype(np.int32),
        win=win,
        anorm=np.ascontiguousarray(inp["attn_norm"][l].reshape(8, 128).T),
        qkg=np.ascontiguousarray(qkg),
        slopes=slopes,
        ident=np.eye(128, dtype=np.float32),
        blk64=blk,
        tri=np.triu(np.ones((128, 128), np.float32)),
        mem=np.ascontiguousarray(inp["mem"][b]),
        wkv=np.ascontiguousarray(inp["w_mem_kv"][l][:, [h * 64 + d for h in heads_mem for d in range(64)] + [256 + h * 64 + d for h in heads_mem for d in range(64)]]),
        mnorm=np.ascontiguousarray(inp["mem_norm"][l].reshape(8, 128).T),
        wout=np.ascontiguousarray(inp["w_out"][l][mixrows]),
        wosc=np.ascontiguousarray(np.concatenate([np.tile(inp["da_subln"][l], len(heads_da)), np.ones(1024 - 128 * len(heads_da), np.float32)]).reshape(8, 128).T),
        fnorm=np.ascontiguousarray(inp["ffn_norm"][l].reshape(8, 128).T),
        rwp=_rwp(inp, heads_rw), lmu=_lmu(inp),
        w2=np.ascontiguousarray(inp["rw_w2"][l][:, [h * 64 + c for h in heads_rw for c in range(64)]]),
        a2=np.ascontiguousarray(inp["rw_a2"][l][:, [h * 64 + c for h in heads_rw for c in range(64)]]),
        g2=np.ascontiguousarray(inp["rw_g2"][l][:, [h * 64 + c for h in heads_rw for c in range(64)]]),
        mask4=_MASK4, masksl=_MASKSL, rmask=_RMASK,
        rbias=np.concatenate([inp["b_group_router"][l], inp["b_expert_router"][l]]).astype(np.float32),
        wr=np.ascontiguousarray(np.concatenate([inp["w_group_router"][l], inp["w_expert_router"][l]], axis=1)),
        wge=inp["w_e_gate"][l], wue=inp["w_e_up"][l], wde=inp["w_e_down"][l],
        xo=np.ascontiguousarray(inp["x"][b][tail_tok0:tail_tok0 + cfg.TT]),
        mkg=np.ascontiguousarray(np.concatenate([inp["mem_k_norm"][l], inp["mem_k_norm"][l]]).reshape(128, 1)),
        lamv=np.stack([inp["da_lambda_q1"][l], inp["da_lambda_k1"][l], inp["da_lambda_q2"][l], inp["da_lambda_k2"][l]]).astype(np.float32),
    )


def kernel(**inputs):
    inp = {k: np.asarray(v) for k, v in inputs.items()}
    cfg = Cfg()
    nc = build(cfg)
    in_maps = []
    for c in range(8):
        b = c % 4
        in_maps.append(_core_inputs(cfg, inp, b, [0, 1, 2, 3], [0, 1, 2, 3], [0, 1, 2, 3]))
    res = run_bass_kernel_spmd(nc, in_maps, core_ids=list(range(8)))
    out = np.stack([res.results[b]["out"] for b in range(4)], axis=0)
    return out
```
